# Optimizing a Trainium2 kernel written in Bass

```python
import math
import jax, jax.numpy as jnp
from jax import lax
import numpy as np

D_MODEL = 1024
BATCH = 8
SEQ = 4096
DEPTH = 1

RET_HEADS = 4
RET_DK = 128
RET_DV = 256
RET_CHUNK = 128
DIFF_HEADS = 4
DIFF_DK = 128
DIFF_DV = 256
Q_BLOCK = 128
ROPE_THETA = 10000.0
N_GROUPS = 4
EXPERTS_PER_GROUP = 8
N_EXPERTS = 32
TOP_K = 2
D_EXPERT = 512
MOE_BLOCK = 128
EPS = 1e-6

RET_QK = 512
RET_V = 1024
DIFF_QK = 1024
DIFF_V = 1024
IN_OFFSETS = (512, 1024, 2048, 3072, 4096, 5120, 6144, 7168)
D_IN = 8192

kernel_name = "hybrid_retention_diffattn_hmoe_block"


def rmsnorm(x, g):
    xf = x.astype(jnp.float32)
    y = xf * lax.rsqrt(jnp.mean(xf * xf, axis=-1, keepdims=True) + EPS)
    return (y * g.astype(jnp.float32)).astype(x.dtype)


def head_rms(o):
    of = o.astype(jnp.float32)
    return of * lax.rsqrt(jnp.mean(of * of, axis=-1, keepdims=True) + EPS)


def apply_rope(t, positions):
    d = t.shape[-1]
    inv = ROPE_THETA ** (-jnp.arange(0, d, 2, dtype=jnp.float32) / d)
    ang = positions.astype(jnp.float32)[:, :, None] * inv
    cos = jnp.cos(ang)[:, :, None, :]
    sin = jnp.sin(ang)[:, :, None, :]
    tf = t.astype(jnp.float32)
    t1, t2 = tf[..., : d // 2], tf[..., d // 2:]
    return jnp.concatenate([t1 * cos - t2 * sin, t2 * cos + t1 * sin], axis=-1).astype(t.dtype)


def retention(q, k, v):
    B, S, H, dk = q.shape
    dv = v.shape[-1]
    C = RET_CHUNK
    N = S // C
    gamma = 1.0 - jnp.exp2(-5.0 - jnp.arange(H, dtype=jnp.float32))
    log_g = jnp.log(gamma)
    idx = jnp.arange(C, dtype=jnp.float32)
    rel = idx[:, None] - idx[None, :]
    dmask = jnp.where(rel >= 0, jnp.exp(log_g[:, None, None] * jnp.maximum(rel, 0.0)), 0.0)
    zeta = jnp.exp(log_g[:, None] * (C - 1 - idx)).T
    xi = jnp.exp(log_g[:, None] * (idx + 1)).T
    chunk_decay = jnp.exp(log_g * C)
    qc = q.astype(jnp.float32).reshape(B, N, C, H, dk)
    kc = k.astype(jnp.float32).reshape(B, N, C, H, dk) * (dk ** -0.5)
    vc = v.astype(jnp.float32).reshape(B, N, C, H, dv)
    s = jnp.einsum('bnihd,bnjhd->bnhij', qc, kc) * dmask
    o_in = jnp.einsum('bnhij,bnjhe->bnihe', s, vc)
    kv = jnp.einsum('bnjhd,bnjhe->bnhde', kc * zeta[None, None, :, :, None], vc)

    def step(state, kv_n):
        return chunk_decay[None, :, None, None] * state + kv_n, state

    _, state_prev = lax.scan(step, jnp.zeros((B, H, dk, dv), jnp.float32), jnp.swapaxes(kv, 0, 1))
    o_cross = jnp.einsum('bnihd,nbhde->bnihe', qc * xi[None, None, :, :, None], state_prev)
    return (o_in + o_cross).reshape(B, S, H, dv)


def diff_attention(q, k, v, lam):
    B, S, H, _, dk = q.shape
    dv = v.shape[-1]
    nq = S // Q_BLOCK
    qb = jnp.moveaxis(q.reshape(B, nq, Q_BLOCK, H, 2, dk), 1, 0)
    kpos = jnp.arange(S)
    scale = dk ** -0.5
    neg = jnp.finfo(jnp.float32).min

    def block(args):
        i, qi = args
        s = jnp.einsum('bqhcd,bkhcd->bhcqk', qi, k).astype(jnp.float32) * scale
        qpos = i * Q_BLOCK + jnp.arange(Q_BLOCK)
        s = jnp.where(kpos[None, :] <= qpos[:, None], s, neg)
        p = jax.nn.softmax(s, axis=-1)
        a = p[:, :, 0] - lam * p[:, :, 1]
        return jnp.einsum('bhqk,bkhe->bqhe', a.astype(v.dtype), v)

    o = lax.map(block, (jnp.arange(nq), qb))
    return jnp.moveaxis(o, 0, 1).reshape(B, S, H, dv)


def hierarchical_moe(h, w_rg, b_rg, w_re, b_re, w1, w3, w2):
    T, D = h.shape
    gp = jax.nn.softmax(jnp.dot(h, w_rg).astype(jnp.float32) + b_rg, axis=-1)
    g_top, g_idx = lax.top_k(gp, 1)
    el_all = (jnp.dot(h, w_re).astype(jnp.float32) + b_re).reshape(T, N_GROUPS, EXPERTS_PER_GROUP)
    el = jnp.take_along_axis(el_all, g_idx[:, :, None], axis=1)[:, 0]
    e_val, e_idx = lax.top_k(el, TOP_K)
    weights = (g_top * jax.nn.softmax(e_val, axis=-1)).astype(h.dtype)
    expert = g_idx * EXPERTS_PER_GROUP + e_idx

    flat_e = expert.reshape(-1)
    A = flat_e.shape[0]
    order = jnp.argsort(flat_e)
    sorted_e = flat_e[order]
    counts = jnp.bincount(flat_e, length=N_EXPERTS)
    starts = jnp.cumsum(counts) - counts
    padded = ((counts + MOE_BLOCK - 1) // MOE_BLOCK) * MOE_BLOCK
    pad_ends = jnp.cumsum(padded)
    pad_starts = pad_ends - padded
    dest = pad_starts[sorted_e] + (jnp.arange(A) - starts[sorted_e])
    n_blocks = -(-A // MOE_BLOCK) + N_EXPERTS
    P = n_blocks * MOE_BLOCK
    row_token = jnp.full((P,), T, jnp.int32).at[dest].set((order // TOP_K).astype(jnp.int32))
    block_e = jnp.minimum(jnp.searchsorted(pad_ends, jnp.arange(n_blocks) * MOE_BLOCK, side='right'),
                          N_EXPERTS - 1)
    h_pad = jnp.concatenate([h, jnp.zeros((1, D), h.dtype)], axis=0)[row_token]
    h_pad = h_pad.reshape(n_blocks, MOE_BLOCK, D)

    def expert_block(args):
        xb, e = args
        return jnp.dot(jax.nn.silu(jnp.dot(xb, w1[e])) * jnp.dot(xb, w3[e]), w2[e])

    y_pad = lax.map(expert_block, (h_pad, block_e)).reshape(P, D)
    dest_of_flat = jnp.zeros((A,), jnp.int32).at[order].set(dest.astype(jnp.int32))
    y_assign = y_pad[dest_of_flat].reshape(T, TOP_K, D)
    return jnp.sum(y_assign * weights[:, :, None], axis=1)


def setup_inputs(seed: int = 0) -> dict:
    key = jax.random.key(seed)
    ks = jax.random.split(key, 24)
    f32 = jnp.float32
    D = D_MODEL

    def nrm(k, shape, scale):
        return jax.random.normal(k, shape, f32) * scale

    return {
        "x": nrm(ks[0], (BATCH, SEQ, D), 1.0),
        "c": nrm(ks[1], (BATCH, D), 1.0),
        "positions": jnp.broadcast_to(jnp.arange(SEQ, dtype=jnp.int32)[None, :], (BATCH, SEQ)),
        "w_ada": nrm(ks[2], (DEPTH, D, 6 * D), D ** -0.5),
        "b_ada": nrm(ks[3], (DEPTH, 6 * D), 0.02),
        "norm_mix": 1.0 + nrm(ks[4], (DEPTH, D), 0.02),
        "w_in": nrm(ks[5], (DEPTH, D, D_IN), D ** -0.5),
        "w_ret_o": nrm(ks[6], (DEPTH, RET_V, D), RET_V ** -0.5),
        "w_diff_o": nrm(ks[7], (DEPTH, DIFF_V, D), DIFF_V ** -0.5),
        "lam_q1": nrm(ks[8], (DEPTH, DIFF_DK), 0.1),
        "lam_k1": nrm(ks[9], (DEPTH, DIFF_DK), 0.1),
        "lam_q2": nrm(ks[10], (DEPTH, DIFF_DK), 0.1),
        "lam_k2": nrm(ks[11], (DEPTH, DIFF_DK), 0.1),
        "diff_norm": 1.0 + nrm(ks[12], (DEPTH, DIFF_DV), 0.02),
        "w_out": nrm(ks[13], (DEPTH, D, D), D ** -0.5),
        "norm_ffn": 1.0 + nrm(ks[14], (DEPTH, D), 0.02),
        "w_router_group": nrm(ks[15], (DEPTH, D, N_GROUPS), D ** -0.5),
        "b_router_group": nrm(ks[16], (DEPTH, N_GROUPS), 0.01),
        "w_router_expert": nrm(ks[17], (DEPTH, D, N_GROUPS * EXPERTS_PER_GROUP), D ** -0.5),
        "b_router_expert": nrm(ks[18], (DEPTH, N_GROUPS * EXPERTS_PER_GROUP), 0.01),
        "w_exp_gate": nrm(ks[19], (DEPTH, N_EXPERTS, D, D_EXPERT), D ** -0.5),
        "w_exp_up": nrm(ks[20], (DEPTH, N_EXPERTS, D, D_EXPERT), D ** -0.5),
        "w_exp_down": nrm(ks[21], (DEPTH, N_EXPERTS, D_EXPERT, D), D_EXPERT ** -0.5),
        "norm_final": 1.0 + nrm(ks[22], (D,), 0.02),
    }


def reference(x, c, positions, w_ada, b_ada, norm_mix, w_in, w_ret_o, w_diff_o,
              lam_q1, lam_k1, lam_q2, lam_k2, diff_norm, w_out, norm_ffn,
              w_router_group, b_router_group, w_router_expert, b_router_expert,
              w_exp_gate, w_exp_up, w_exp_down, norm_final):
    B, S, D = x.shape
    for l in range(DEPTH):
        lambda_init = 0.8 - 0.6 * math.exp(-0.3 * l)
        mod = (jnp.dot(jax.nn.silu(c), w_ada[l]) + b_ada[l])[:, None, :]
        sh1, sc1, g1, sh2, sc2, g2 = jnp.split(mod, 6, axis=-1)

        h = rmsnorm(x, norm_mix[l]) * (1.0 + sc1) + sh1
        proj = jnp.dot(h, w_in[l])
        rq, rk, rv, rg, dq, dk, dv, gate_r, gate_d = jnp.split(proj, list(IN_OFFSETS), axis=-1)

        rq = apply_rope(rq.reshape(B, S, RET_HEADS, RET_DK), positions)
        rk = apply_rope(rk.reshape(B, S, RET_HEADS, RET_DK), positions)
        ro = retention(rq, rk, rv.reshape(B, S, RET_HEADS, RET_DV))
        ro = head_rms(ro).reshape(B, S, RET_V).astype(x.dtype) * jax.nn.silu(rg)
        ret_out = jnp.dot(ro, w_ret_o[l])

        dq = apply_rope(dq.reshape(B, S, 2 * DIFF_HEADS, DIFF_DK), positions).reshape(B, S, DIFF_HEADS, 2, DIFF_DK)
        dk = apply_rope(dk.reshape(B, S, 2 * DIFF_HEADS, DIFF_DK), positions).reshape(B, S, DIFF_HEADS, 2, DIFF_DK)
        lam = (jnp.exp(jnp.sum(lam_q1[l].astype(jnp.float32) * lam_k1[l].astype(jnp.float32)))
               - jnp.exp(jnp.sum(lam_q2[l].astype(jnp.float32) * lam_k2[l].astype(jnp.float32)))
               + lambda_init)
        do = diff_attention(dq, dk, dv.reshape(B, S, DIFF_HEADS, DIFF_DV), lam)
        do = head_rms(do) * diff_norm[l].astype(jnp.float32) * (1.0 - lambda_init)
        diff_out = jnp.dot(do.reshape(B, S, DIFF_V).astype(x.dtype), w_diff_o[l])

        merged = jax.nn.sigmoid(gate_r) * ret_out + jax.nn.sigmoid(gate_d) * diff_out
        x = x + g1 * jnp.dot(merged, w_out[l])

        h2 = rmsnorm(x, norm_ffn[l]) * (1.0 + sc2) + sh2
        y = hierarchical_moe(h2.reshape(B * S, D), w_router_group[l], b_router_group[l],
                             w_router_expert[l], b_router_expert[l],
                             w_exp_gate[l], w_exp_up[l], w_exp_down[l]).reshape(B, S, D)
        x = x + g2 * y
    return rmsnorm(x, norm_final)
```

```python
import math
from contextlib import ExitStack

import numpy as np
import ml_dtypes
import concourse.bass as bass
import concourse.mybir as mybir
from concourse.bass_utils import run_bass_kernel_spmd

F32 = mybir.dt.float32
BF16 = mybir.dt.bfloat16
I32 = mybir.dt.int32
AF = mybir.ActivationFunctionType
ALU = mybir.AluOpType
AX = mybir.AxisListType

S_LEN = 4096
D = 1024
NT = 32
NG = 8
EPS = 1e-6
LAMBDA_INIT = 0.8 - 0.6 * math.exp(0.0)
MAGIC = 12582912.0
N_EXP = 32
D_EXP = 512


class Buf:
    def __init__(self, name=""):
        self.name = name
        self.w = []
        self.r = []
        self.dsem = None


class TB:
    def __init__(self, t, name):
        self.t = t
        self.b = Buf(name)

    def __getitem__(self, k):
        return self.t[k]


class Sched:
    def __init__(self, nc, es):
        self.nc = nc
        self.es = es
        self.engs = ["pe", "act", "dve", "pool", "sp"]
        self.prog = {k: [] for k in self.engs}
        self.sems = {}
        self.cnt = {}
        self.waited = {k: {} for k in self.engs}
        for k in self.engs:
            self._mksem("E_" + k)
        self.nd = 0

    def _mksem(self, name):
        s = self.es.enter_context(self.nc.semaphore(name))
        self.sems[name] = s
        self.cnt[name] = 0
        return name

    def _deps(self, eng, reads, writes):
        deps = {}
        for b in reads:
            for (s, v) in b.w:
                deps[s] = max(deps.get(s, 0), v)
        for b in writes:
            for (s, v) in b.w + b.r:
                deps[s] = max(deps.get(s, 0), v)
        out = []
        for s, v in deps.items():
            if eng == "pe" and s == "E_pe":
                continue
            if self.waited[eng].get(s, 0) >= v:
                continue
            self.waited[eng][s] = v
            out.append((s, v))
        return out

    def _commit(self, tok, reads, writes, acc=False):
        for b in reads:
            b.r.append(tok)
            if len(b.r) > 64:
                m = {}
                for (s, v) in b.r:
                    m[s] = max(m.get(s, 0), v)
                b.r = list(m.items())
        for b in writes:
            if acc:
                b.w.append(tok)
            else:
                b.w = [tok]
            b.r = []

    @staticmethod
    def _bl(xs):
        return [x.b if isinstance(x, TB) else x for x in xs]

    def op(self, eng, fn, reads=(), writes=()):
        reads = self._bl(reads)
        writes = self._bl(writes)
        waits = self._deps(eng, reads, writes)
        sname = "E_" + eng
        self.cnt[sname] += 1
        val = self.cnt[sname]
        sems = self.sems

        def run(e, waits=waits, fn=fn, sname=sname):
            for (s, v) in waits:
                e.wait_ge(sems[s], v)
            ins = fn(e)
            ins.then_inc(sems[sname], 1)

        self.prog[eng].append(run)
        self._commit((sname, val), reads, writes)

    def dma(self, eng, out, in_, slot, reads=(), writes=(), acc=False, track=(), **kw):
        reads = self._bl(reads)
        writes = self._bl(writes)
        track = self._bl(track)
        slot = slot.b if isinstance(slot, TB) else slot
        if slot.dsem is None:
            self.nd += 1
            slot.dsem = self._mksem("D%d" % self.nd)
        sname = slot.dsem
        waits = self._deps(eng, reads, writes)
        self.cnt[sname] += 16
        val = self.cnt[sname]
        sems = self.sems

        def run(e, waits=waits, sname=sname):
            for (s, v) in waits:
                e.wait_ge(sems[s], v)
            e.dma_start(out=out, in_=in_, **kw).then_inc(sems[sname], 16)

        self.prog[eng].append(run)
        self._commit((sname, val), reads, writes, acc=acc)
        for b in track:
            b.w.append((sname, val))

    def custom(self, eng, fn, slot, n_dma, reads=(), writes=(), acc=False, track=()):
        reads = self._bl(reads)
        writes = self._bl(writes)
        track = self._bl(track)
        slot = slot.b if isinstance(slot, TB) else slot
        if slot.dsem is None:
            self.nd += 1
            slot.dsem = self._mksem("D%d" % self.nd)
        sname = slot.dsem
        waits = self._deps(eng, reads, writes)
        self.cnt[sname] += 16 * n_dma
        val = self.cnt[sname]
        sems = self.sems

        def run(e, waits=waits, sname=sname):
            for (s, v) in waits:
                e.wait_ge(sems[s], v)
            fn(e, sems[sname])

        self.prog[eng].append(run)
        self._commit((sname, val), reads, writes, acc=acc)
        for b in track:
            b.w.append((sname, val))

    def barrier(self):
        snap = dict(self.cnt)
        sems = self.sems
        for eng in self.engs:
            waits = []
            for s, v in snap.items():
                if v == 0:
                    continue
                if eng == "pe" and s == "E_pe":
                    continue
                if self.waited[eng].get(s, 0) >= v:
                    continue
                self.waited[eng][s] = v
                waits.append((s, v))

            def run(e, waits=waits):
                for (s, v) in waits:
                    e.wait_ge(sems[s], v)
            self.prog[eng].append(run)

    def emit(self):
        nc = self.nc
        prog = self.prog
        with nc.Block() as block:
            @block.tensor
            def _(e):
                for f in prog["pe"]:
                    f(e)

            @block.scalar
            def _(e):
                for f in prog["act"]:
                    f(e)

            @block.vector
            def _(e):
                for f in prog["dve"]:
                    f(e)

            @block.gpsimd
            def _(e):
                for f in prog["pool"]:
                    f(e)

            @block.sync
            def _(e):
                for f in prog["sp"]:
                    f(e)
        self.prog = {k: [] for k in self.engs}


def host_consts():
    c = {}
    c["ident_bf"] = np.eye(128, dtype=np.float32).astype(ml_dtypes.bfloat16)
    c["ident_f"] = np.eye(128, dtype=np.float32)
    sw = np.zeros((128, 128), np.float32)
    for dp in range(128):
        sw[(dp + 64) % 128, dp] = 1.0
    c["swap_bf"] = sw.astype(ml_dtypes.bfloat16)
    d = np.arange(128)
    inv = 10000.0 ** (-(2.0 * (d % 64)) / 128.0)
    sgn = np.where(d < 64, -1.0, 1.0)
    vec = np.zeros((128, 4), np.float32)
    vec[:, 0] = inv.astype(np.float32)
    vec[:, 1] = sgn
    vec[:, 2] = d
    vec[:, 3] = np.where(d > 0, 1.0e5, 0.0)
    c["vec"] = vec
    c["ltri"] = (d[:, None] < d[None, :]).astype(np.float32)
    c["ones_f"] = np.ones((128, 128), np.float32)
    c["kcoff"] = (np.arange(8)[None, :] * 128 + d[:, None]).astype(np.float32)
    c["iota_b"] = np.tile(np.arange(128, dtype=np.float32)[None, :], (128, 1))
    H = 4
    gam = 1.0 - 2.0 ** (-5.0 - np.arange(H))
    lg = np.log(gam)
    idx = np.arange(128)
    scale = 128.0 ** -0.5
    dm = np.zeros((128, H, 128), np.float32)
    for h in range(H):
        rel = idx[None, :] - idx[:, None]
        dm[:, h, :] = np.where(rel >= 0, np.exp(lg[h] * np.maximum(rel, 0)), 0.0) * scale
    c["dmaskT"] = dm
    zeta = np.zeros((128, H), np.float32)
    for h in range(H):
        zeta[:, h] = np.exp(lg[h] * (127 - idx)) * scale
    c["zeta"] = zeta
    xi = np.zeros((128, H, 512), np.float32)
    for h in range(H):
        xi[:, h, :] = np.tile(np.exp(lg[h] * (idx + 1)), 4)[None, :]
    c["xi"] = xi.astype(ml_dtypes.bfloat16)
    c["_cdecay"] = [float(np.exp(lg[h] * 128)) for h in range(H)]
    tri = (idx[:, None] <= idx[None, :]).astype(np.float32)
    c["tri_bf"] = tri.astype(ml_dtypes.bfloat16)
    return c


CONSTS = host_consts()


def build(debug=False, stop=99):
    nc = bass.Bass("TRN2", target_bir_lowering=False)
    cdecay = CONSTS["_cdecay"]

    def din(name, shape, dt):
        return nc.dram_tensor(name, list(shape), dt, kind="ExternalInput").ap()

    x_d = din("x", [S_LEN, D], F32)
    c_d = din("c", [128, 8], F32)
    pos_d = din("positions", [1, S_LEN], I32)
    w_ada = din("w_ada", [D, 6 * D], F32)
    aux_d = din("aux", [1, 6 * D + 3 * D + 256 + 36], F32)
    w_in = din("w_in", [D, 8192], F32)
    w_ret_o = din("w_ret_o", [D, D], F32)
    w_diff_o = din("w_diff_o", [D, D], F32)
    w_out = din("w_out", [D, D], F32)
    lam_d = din("lam", [4, 128], F32)
    w_rt = din("w_rt", [D, 36], F32)
    w_eg_rows = din("w_exp_gate", [N_EXP * 128, 8 * D_EXP], F32)
    w_eu_rows = din("w_exp_up", [N_EXP * 128, 8 * D_EXP], F32)
    w_ed_rows = din("w_exp_down", [N_EXP * 128, 4 * D], F32)
    k_ident_bf = din("ident_bf", [128, 128], BF16)
    k_ident_f = din("ident_f", [128, 128], F32)
    k_swap = din("swap_bf", [128, 128], BF16)
    k_vec = din("vec", [128, 4], F32)
    k_dmask = din("dmaskT", [128, 4, 128], F32)
    k_zeta = din("zeta", [128, 4], F32)
    k_xi = din("xi", [128, 4, 512], BF16)
    k_tri = din("tri_bf", [128, 128], BF16)
    k_ltri = din("ltri", [128, 128], F32)
    k_ones = din("ones_f", [128, 128], F32)
    k_kcoff = din("kcoff", [128, 8], F32)
    k_iota = din("iota_b", [128, 128], F32)

    out_d = nc.dram_tensor("out", [S_LEN, D], F32, kind="ExternalOutput").ap()
    skind = "ExternalOutput" if debug else "Internal"

    def dscr(name, shape, dt):
        return nc.dram_tensor(name, list(shape), dt, kind=skind).ap()

    hT_d = dscr("hT_d", [128, 8, S_LEN], BF16)
    roT_d = dscr("roT_d", [128, 8, S_LEN], BF16)
    doT_d = dscr("doT_d", [128, 8, S_LEN], BF16)
    mT_d = dscr("mT_d", [128, 8, S_LEN], BF16)
    NSUB = 2
    RB = 128 * NSUB
    NBIG = (2 * S_LEN) // RB + N_EXP
    NBLK = NBIG * NSUB
    xpad_d = dscr("xpad_d", [NBLK * 128, D], BF16)
    ypad_d = dscr("ypad_d", [NBLK * 128, D], F32)
    B_xpad, B_ypad = Buf("xpad"), Buf("ypad")
    B_xz = Buf("xpad_zero")
    wb_eg = dscr("wb_eg", [N_EXP * 128, 8 * D_EXP], BF16)
    wb_eu = dscr("wb_eu", [N_EXP * 128, 8 * D_EXP], BF16)
    wb_ed = dscr("wb_ed", [N_EXP * 128, 4 * D], BF16)
    B_wb = Buf("wb")
    cvt_list = [(dst, src, r) for (dst, src) in [(wb_eg, w_eg_rows), (wb_eu, w_eu_rows), (wb_ed, w_ed_rows)] for r in range(N_EXP)]
    cvt_pos = [0]
    x1_d = dscr("x1_d", [S_LEN, D], F32)
    B_hT, B_roT, B_doT, B_mT, B_h2T, B_x1, B_out, B_wd = (Buf(n) for n in
                                                         ["hT", "roT", "doT", "mT", "h2T", "x1", "out", "wd"])

    with ExitStack() as es:
        S = Sched(nc, es)
        uid = [0]

        def cvt_issue(n):
            for _ in range(n):
                if cvt_pos[0] >= len(cvt_list):
                    return
                dst, src, r = cvt_list[cvt_pos[0]]
                cvt_pos[0] += 1
                S.dma("pool", dst[r * 128:(r + 1) * 128, :], src[r * 128:(r + 1) * 128, :], B_wb, writes=[B_wb], acc=True)

        def sb(stack, shape, dt, name=None):
            uid[0] += 1
            nm = (name or "t") + "_%d" % uid[0]
            return TB(stack.enter_context(nc.sbuf_tensor(nm, list(shape), dt)), nm)

        def ps(stack, shape, dt, name=None):
            uid[0] += 1
            nm = (name or "p") + "_%d" % uid[0]
            return TB(stack.enter_context(nc.psum_tensor(nm, list(shape), dt)), nm)

        ident = sb(es, [128, 128], BF16, "ident")
        identf = sb(es, [128, 128], F32, "identf")
        vec = sb(es, [128, 4], F32, "vec")
        modT = sb(es, [128, 64], F32, "modT")
        A1 = sb(es, [128, 8], F32, "A1")
        A2 = sb(es, [128, 8], F32, "A2")
        g1b = sb(es, [128, D], F32, "g1b")
        g2b = sb(es, [128, D], F32, "g2b")
        nfb = sb(es, [128, D], F32, "nfb")
        dnb = sb(es, [128, 256], F32, "dnb")
        rbb = sb(es, [128, 36], F32, "rbb")
        neglam = sb(es, [128, 1], F32, "neglam")
        MM = sb(es, [128, NT, 2, 32], F32, "MM")
        wAB = sb(es, [128, NT, 2], F32, "wAB")
        A2b = sb(es, [128, D], F32, "A2b")
        B2b = sb(es, [128, D], F32, "B2b")
        ltri = sb(es, [128, 128], F32, "ltri")
        onesf = sb(es, [128, 128], F32, "onesf")
        kcoff = sb(es, [128, 8], F32, "kcoff")
        iota_b = sb(es, [128, 128], F32, "iota_b")
        desti = sb(es, [128, NT, 2], I32, "desti")
        widx1 = sb(es, [128, NBIG], I32, "widx1")
        wrt = sb(es, [128, 8, 36], F32, "wrt")
        ones_row = sb(es, [1, 128], F32, "ones_row")
        es_rope = ExitStack()
        swapm = sb(es_rope, [128, 128], BF16, "swap")
        dmask = sb(es_rope, [128, 4, 128], F32, "dmask")
        zeta = sb(es_rope, [128, 4], F32, "zeta")
        xi = sb(es_rope, [128, 4, 512], BF16, "xi")
        tri = sb(es_rope, [128, 128], BF16, "tri")
        cosT = sb(es_rope, [128, S_LEN], BF16, "cosT")
        sinT = sb(es_rope, [128, S_LEN], BF16, "sinT")

        for (t, src) in [(ident, k_ident_bf), (identf, k_ident_f), (swapm, k_swap), (vec, k_vec), (dmask, k_dmask),
                         (zeta, k_zeta), (xi, k_xi), (tri, k_tri), (ltri, k_ltri), (onesf, k_ones), (kcoff, k_kcoff), (iota_b, k_iota)]:
            S.dma("sp", t[:], src, t, writes=[t])
        S.dma("sp", wrt[:], w_rt.rearrange("(kc p) n -> p kc n", p=128), wrt, writes=[wrt])
        S.op("dve", lambda e: e.memset(ones_row[:], 1.0), writes=[ones_row])

        with ExitStack() as ph:
            scol = sb(ph, [128, 8], F32, "scol")
            ccol = sb(ph, [128, 8], F32, "ccol")
            modrow = sb(ph, [1, 6 * D + 3 * D + 256 + 36], F32, "modrow")
            auxrow = sb(ph, [1, 6 * D + 3 * D + 256 + 36], F32, "auxrow")
            wst = [sb(ph, [128, 8, 512], BF16, "wada%d" % i) for i in range(3)]
            scol_bf = sb(ph, [128, 8], BF16, "scol_bf")
            pm = [ps(ph, [1, 512], F32, "pmod%d" % i) for i in range(2)]
            pT = ps(ph, [128, 64], F32, "pT")
            pb = [ps(ph, [128, 512], F32, "pb%d" % i) for i in range(2)]

            S.dma("sp", ccol[:], c_d, ccol, writes=[ccol])
            S.dma("sp", auxrow[:], aux_d, auxrow, writes=[auxrow])
            S.op("act", lambda e: e.activation(out=scol[:], in_=ccol[:], func=AF.Silu), reads=[ccol], writes=[scol])
            S.op("dve", lambda e: e.tensor_copy(scol_bf[:], scol[:]), reads=[scol], writes=[scol_bf])
            HC = 2048
            posi = sb(ph, [128, HC], I32, "posi")
            ang = sb(ph, [128, HC], F32, "ang")
            kk = sb(ph, [128, HC], F32, "kk")
            a2 = sb(ph, [128, HC], F32, "a2")
            for hc in range(S_LEN // HC):
                sl = slice(hc * HC, (hc + 1) * HC)
                S.dma("sp", posi[:], pos_d[0:1, sl].partition_broadcast(128), posi, writes=[posi])
                S.op("dve", lambda e: e.tensor_copy(ang[:], posi[:]), reads=[posi], writes=[ang])
                S.op("dve", lambda e: e.tensor_scalar(out=ang[:], in0=ang[:], scalar1=vec[:, 0:1], scalar2=None,
                                                      op0=ALU.mult), reads=[ang, vec], writes=[ang])
                for which in range(2):
                    dst = sinT if which == 0 else cosT
                    if which == 1:
                        S.op("dve", lambda e: e.tensor_scalar(out=ang[:], in0=ang[:], scalar1=float(np.pi / 2),
                                                              scalar2=None, op0=ALU.add), reads=[ang], writes=[ang])
                    S.op("dve", lambda e: e.tensor_scalar(out=kk[:], in0=ang[:], scalar1=float(1 / (2 * np.pi)),
                                                          scalar2=MAGIC, op0=ALU.mult, op1=ALU.add),
                         reads=[ang], writes=[kk])
                    S.op("dve", lambda e: e.tensor_scalar(out=kk[:], in0=kk[:], scalar1=-MAGIC,
                                                          scalar2=float(-2 * np.pi), op0=ALU.add, op1=ALU.mult),
                         reads=[kk], writes=[kk])
                    S.op("dve", lambda e: e.tensor_tensor(out=a2[:], in0=ang[:], in1=kk[:], op=ALU.add),
                         reads=[ang, kk], writes=[a2])
                    S.op("dve", lambda e: e.tensor_scalar(out=a2[:], in0=a2[:], scalar1=float(np.pi),
                                                          scalar2=float(-np.pi), op0=ALU.min, op1=ALU.max),
                         reads=[a2], writes=[a2])
                    if which == 0:
                        S.op("act", lambda e: e.activation(out=a2[:], in_=a2[:], func=AF.Sin), reads=[a2], writes=[a2])
                        S.op("dve", lambda e, sl=sl: e.tensor_scalar(out=sinT[:, sl], in0=a2[:], scalar1=vec[:, 1:2],
                                                                     scalar2=None, op0=ALU.mult),
                             reads=[a2, vec], writes=[sinT])
                    else:
                        S.op("act", lambda e, sl=sl: e.activation(out=cosT[:, sl], in_=a2[:], func=AF.Sin),
                             reads=[a2], writes=[cosT])
            w_ada_v = w_ada.rearrange("(kc p) n -> p kc n", p=128)
            for j in range(12):
                w = wst[j % 3]
                S.dma("pool", w[:], w_ada_v[:, :, j * 512:(j + 1) * 512], w, writes=[w])
                p = pm[j % 2]

                def mm(e, w=w, p=p):
                    for k in range(8):
                        ins = e.matmul(p[:], lhsT=scol_bf[:, k:k + 1], rhs=w[:, k, :], start=(k == 0), stop=(k == 7))
                    return ins
                S.op("pe", mm, reads=[scol_bf, w], writes=[p])
                S.op("dve", lambda e, p=p, j=j: e.tensor_tensor(out=modrow[:, j * 512:(j + 1) * 512], in0=p[:],
                                                                in1=auxrow[:, j * 512:(j + 1) * 512], op=ALU.add),
                     reads=[p, auxrow], writes=[modrow])
            S.op("dve", lambda e: e.tensor_copy(modrow[:, 6 * D:], auxrow[:, 6 * D:]), reads=[auxrow], writes=[modrow])

            def mmT(e):
                for j in range(64):
                    ins = e.matmul(pT[:, j:j + 1], lhsT=modrow[:, j * 128:(j + 1) * 128], rhs=ones_row[:, 0:1],
                                   start=True, stop=True)
                return ins
            S.op("pe", mmT, reads=[modrow, ones_row], writes=[pT])
            S.op("dve", lambda e: e.tensor_copy(modT[:], pT[:]), reads=[pT], writes=[modT])
            S.op("dve", lambda e: e.scalar_tensor_tensor(out=A1[:], in0=modT[:, 8:16], scalar=1.0, in1=modT[:, 48:56],
                                                         op0=ALU.add, op1=ALU.mult), reads=[modT], writes=[A1])
            S.op("dve", lambda e: e.scalar_tensor_tensor(out=A2[:], in0=modT[:, 32:40], scalar=1.0, in1=modT[:, 56:64],
                                                         op0=ALU.add, op1=ALU.mult), reads=[modT], writes=[A2])

            def bcast(dst, off, n, i, mul=None):
                p = pb[i % 2]

                def mm(e, p=p):
                    return e.matmul(p[:, 0:n], lhsT=ones_row[:, :], rhs=modrow[:, off:off + n], start=True, stop=True)
                S.op("pe", mm, reads=[modrow, ones_row], writes=[p])
                if mul is None:
                    S.op("dve", lambda e, p=p: e.tensor_copy(dst, p[:, 0:n]), reads=[p], writes=[])
                else:
                    S.op("dve", lambda e, p=p: e.tensor_scalar(out=dst, in0=p[:, 0:n], scalar1=mul, scalar2=None,
                                                               op0=ALU.mult), reads=[p], writes=[])
            i = 0
            for half in range(2):
                bcast(g1b[:, half * 512:(half + 1) * 512], 2 * D + half * 512, 512, i); i += 1
                bcast(g2b[:, half * 512:(half + 1) * 512], 5 * D + half * 512, 512, i); i += 1
                bcast(nfb[:, half * 512:(half + 1) * 512], 8 * D + half * 512, 512, i); i += 1
            bcast(dnb[:], 9 * D, 256, i, mul=(1.0 - LAMBDA_INIT)); i += 1
            S.op("dve", lambda e: e.scalar_tensor_tensor(out=modrow[:, 4 * D:5 * D], in0=modrow[:, 4 * D:5 * D], scalar=1.0,
                                                         in1=modrow[:, 7 * D:8 * D], op0=ALU.add, op1=ALU.mult),
                 reads=[modrow, pT], writes=[modrow])
            for half in range(2):
                bcast(A2b[:, half * 512:(half + 1) * 512], 4 * D + half * 512, 512, i); i += 1
                bcast(B2b[:, half * 512:(half + 1) * 512], 3 * D + half * 512, 512, i); i += 1
            bcast(rbb[:], 9 * D + 256, 36, i); i += 1

            lamt = sb(ph, [128, 4, 128], F32, "lamt")
            lprod = sb(ph, [128, 2, 128], F32, "lprod")
            lsum = sb(ph, [128, 2], F32, "lsum")
            for r in range(4):
                S.dma("sp", lamt[:, r, :], lam_d[r:r + 1, :].partition_broadcast(128), lamt, writes=[lamt], acc=(r > 0))
            S.op("dve", lambda e: e.tensor_tensor(out=lprod[:, 0, :], in0=lamt[:, 0, :], in1=lamt[:, 1, :], op=ALU.mult),
                 reads=[lamt], writes=[lprod])
            S.op("dve", lambda e: e.tensor_tensor(out=lprod[:, 1, :], in0=lamt[:, 2, :], in1=lamt[:, 3, :], op=ALU.mult),
                 reads=[lamt, lprod], writes=[lprod])
            S.op("dve", lambda e: e.reduce_sum(out=lsum[:], in_=lprod[:], axis=AX.X), reads=[lprod], writes=[lsum])
            S.op("act", lambda e: e.activation(out=lsum[:], in_=lsum[:], func=AF.Exp), reads=[lsum], writes=[lsum])
            S.op("dve", lambda e: e.tensor_tensor(out=neglam[:], in0=lsum[:, 1:2], in1=lsum[:, 0:1], op=ALU.subtract),
                 reads=[lsum], writes=[neglam])
            S.op("dve", lambda e: e.tensor_scalar(out=neglam[:], in0=neglam[:], scalar1=-LAMBDA_INIT, scalar2=None,
                                                  op0=ALU.add), reads=[neglam], writes=[neglam])

            S.barrier()
            S.emit()

        def rstd_ops(ss, n, lnexp=False):
            S.op("dve", lambda e: e.tensor_scalar(out=ss[:], in0=ss[:], scalar1=1.0 / n, scalar2=EPS,
                                                  op0=ALU.mult, op1=ALU.add), reads=[ss], writes=[ss])
            if lnexp:
                S.op("act", lambda e: e.activation(out=ss[:], in_=ss[:], func=AF.Ln), reads=[ss], writes=[ss])
                S.op("act", lambda e: e.activation(out=ss[:], in_=ss[:], func=AF.Exp, scale=-0.5), reads=[ss], writes=[ss])
            else:
                S.op("act", lambda e: e.activation(out=ss[:], in_=ss[:], func=AF.Sqrt), reads=[ss], writes=[ss])
                S.op("dve", lambda e: e.reciprocal(out=ss[:], in_=ss[:]), reads=[ss], writes=[ss])

        es_w = ExitStack()
        Wh = [sb(es_w, [128, 8, 768], BF16, "Wh%d" % i) for i in range(2)]
        hTg = [sb(es_w, [128, 8, 512], BF16, "hTg%d" % i) for i in range(2)]
        w_in_v = w_in.rearrange("(kc p) n -> p kc n", p=128)

        def load_cols(W, cols):
            off = 0
            for ci, (c0, cn) in enumerate(cols):
                S.dma("pool", W[:, :, off:off + cn], w_in_v[:, :, c0:c0 + cn], W, writes=[W], acc=(ci > 0))
                off += cn

        def cols_ret(h):
            return [(h * 128, 128), (512 + h * 128, 128), (1024 + h * 256, 256), (2048 + h * 256, 256)]

        def cols_diff(h):
            return [(3072 + (2 * h) * 128, 256), (4096 + (2 * h) * 128, 256), (5120 + h * 256, 256)]
        load_cols(Wh[0], cols_ret(0))

        with ExitStack() as ph:
            xt = [sb(ph, [128, D], F32, "xt%d" % i) for i in range(3)]
            junk = sb(ph, [128, D], BF16, "junk")
            xn = [sb(ph, [128, D], BF16, "xn%d" % i) for i in range(3)]
            ssq = [sb(ph, [128, 1], F32, "ss%d" % i) for i in range(3)]
            pT = [ps(ph, [128, 8, 128], BF16, "pT%d" % i) for i in range(3)]
            hg = [sb(ph, [128, 8, 512], BF16, "hg%d" % i) for i in range(2)]
            def p1_a(t):
                x_ = xt[t % 3]
                ss = ssq[t % 3]
                xn_ = xn[t % 3]
                S.dma("sp", x_[:], x_d[t * 128:(t + 1) * 128, :], x_, writes=[x_])
                S.op("act", lambda e: e.activation(out=junk[:], in_=x_[:], func=AF.Square, accum_out=ss[:]),
                     reads=[x_], writes=[junk, ss])
                rstd_ops(ss, D)
                S.op("dve", lambda e: e.tensor_scalar(out=xn_[:], in0=x_[:], scalar1=ss[:, 0:1], scalar2=None, op0=ALU.mult),
                     reads=[x_, ss], writes=[xn_])

            def p1_b(t):
                xn_ = xn[t % 3]
                p_ = pT[t % 3]
                g = t // 4
                hg_ = hg[g % 2]

                def tr(e):
                    for c in range(8):
                        ins = e.transpose(p_[:, c, :], xn_[:, c * 128:(c + 1) * 128], ident[:])
                    return ins
                S.op("pe", tr, reads=[xn_, ident], writes=[p_])
                for c in range(8):
                    if c % 2 == 0:
                        S.op("act", lambda e, c=c: e.activation(
                            out=hg_[:, c, (t % 4) * 128:(t % 4 + 1) * 128], in_=p_[:, c, :], func=AF.Identity,
                            bias=modT[:, c:c + 1], scale=A1[:, c:c + 1]), reads=[p_], writes=[hg_])
                    else:
                        S.op("dve", lambda e, c=c: e.tensor_scalar(
                            out=hg_[:, c, (t % 4) * 128:(t % 4 + 1) * 128], in0=p_[:, c, :], scalar1=A1[:, c:c + 1],
                            scalar2=modT[:, c:c + 1], op0=ALU.mult, op1=ALU.add), reads=[p_], writes=[hg_])
                if t % 4 == 3:
                    S.dma("pool", hT_d[:, :, g * 512:(g + 1) * 512], hg_[:], hg_, reads=[hg_], writes=[B_hT], acc=True)

            p1_a(0)
            p1_a(1)
            for t in range(NT):
                p1_b(t)
                if t + 2 < NT:
                    p1_a(t + 2)
            S.barrier()
            S.emit()
        if stop <= 1:
            S.emit()
            es_w.close()
            es_rope.close()
            return nc

        rope_ctr = [0]

        def rope_evac(ph_tiles, p_in, g, outs, reads_extra=()):
            rope_ctr[0] += 1
            qsb, pswp, t1, t2 = ph_tiles[rope_ctr[0] % len(ph_tiles)]
            sl = slice(g * 512, (g + 1) * 512)
            S.op("act", lambda e: e.activation(out=qsb[:], in_=p_in[:], func=AF.Copy), reads=[p_in], writes=[qsb])
            S.op("pe", lambda e: e.matmul(pswp[:], lhsT=swapm[:], rhs=qsb[:], start=True, stop=True),
                 reads=[qsb, swapm], writes=[pswp])
            S.op("dve", lambda e: e.tensor_tensor(out=t1[:], in0=qsb[:], in1=cosT[:, sl], op=ALU.mult),
                 reads=[qsb], writes=[t1])
            S.op("dve", lambda e: e.tensor_tensor(out=t2[:], in0=pswp[:], in1=sinT[:, sl], op=ALU.mult),
                 reads=[pswp], writes=[t2])
            for (dst, dbuf, mul) in outs:
                if mul is None:
                    S.op("dve", lambda e, dst=dst: e.tensor_tensor(out=dst, in0=t1[:], in1=t2[:], op=ALU.add),
                         reads=[t1, t2], writes=[dbuf])
                else:
                    S.op("dve", lambda e: e.tensor_tensor(out=t1[:], in0=t1[:], in1=t2[:], op=ALU.add),
                         reads=[t1, t2], writes=[t1])
                    S.op("dve", lambda e, dst=dst, mul=mul: e.tensor_tensor(out=dst, in0=t1[:], in1=mul, op=ALU.mult),
                         reads=[t1], writes=[dbuf])

        hT_v = hT_d

        with ExitStack() as ph:
            rqT = sb(ph, [128, S_LEN], BF16, "rqT")
            rqxT = sb(ph, [128, S_LEN], BF16, "rqxT")
            rkT = sb(ph, [128, S_LEN], BF16, "rkT")
            rv = sb(ph, [128, NT, 256], BF16, "rv")
            srgT = sb(ph, [128, 2, S_LEN], BF16, "srgT")
            roT = sb(ph, [128, 2, S_LEN], BF16, "roT")
            qsb = [sb(ph, [128, 512], BF16, "qsb%d" % i) for i in range(2)]
            t1 = [sb(ph, [128, 512], F32, "t1%d" % i) for i in range(2)]
            t2 = [sb(ph, [128, 512], F32, "t2%d" % i) for i in range(2)]
            pA = [ps(ph, [128, 512], F32, "pA%d" % i) for i in range(2)]
            pswp = ps(ph, [128, 512], F32, "pswp")
            pS = ps(ph, [128, 128], F32, "pS")
            pKT = ps(ph, [128, 128], BF16, "pKT")
            pO = ps(ph, [128, 256], F32, "pO")
            pKV = ps(ph, [128, 256], F32, "pKV")
            pRT = ps(ph, [128, 2, 128], BF16, "pRT")
            PT2 = [sb(ph, [128, 128], BF16, "PT%d" % i) for i in range(2)]
            kz2 = [sb(ph, [128, 128], BF16, "kz%d" % i) for i in range(2)]
            state = sb(ph, [128, 256], F32, "state")
            stbf = [sb(ph, [128, 256], BF16, "stbf%d" % i) for i in range(3)]
            on2 = [sb(ph, [128, 256], BF16, "on%d" % i) for i in range(2)]
            ojunk = sb(ph, [128, 256], BF16, "ojunk")
            oss2 = [sb(ph, [128, 1], F32, "oss%d" % i) for i in range(2)]
            rt = [(qsb[i], pswp, t1[i], t2[i]) for i in range(2)]
            pai = [0]

            def nextp():
                pai[0] += 1
                return pA[pai[0] % 2]

            for h in range(4):
                W = Wh[h % 2]
                if h + 1 < 4:
                    load_cols(Wh[(h + 1) % 2], cols_ret(h + 1))
                else:
                    load_cols(Wh[(h + 1) % 2], cols_diff(0))
                for g in range(NG):
                    hT_ = hTg[g % 2]
                    sl = slice(g * 512, (g + 1) * 512)
                    S.dma("sp", hT_[:], hT_v[:, :, sl], hT_, reads=[B_hT], writes=[hT_])
                    cvt_issue(1)

                    def proj_fm(p, c0, W=W, hT_=hT_):
                        def mm(e):
                            for k in range(8):
                                ins = e.matmul(p[:], lhsT=W[:, k, c0:c0 + 128], rhs=hT_[:, k, :], start=(k == 0), stop=(k == 7))
                            return ins
                        S.op("pe", mm, reads=[W, hT_], writes=[p])
                    steps = []
                    steps.append((lambda p: proj_fm(p, 0),
                                  lambda p, g=g, sl=sl, h=h: rope_evac(rt, p, g, [(rqT[:, sl], rqT, None), (rqxT[:, sl], rqxT, xi[:, h, :])])))
                    steps.append((lambda p: proj_fm(p, 128),
                                  lambda p, g=g, sl=sl: rope_evac(rt, p, g, [(rkT[:, sl], rkT, None)])))
                    for c in range(2):
                        steps.append((lambda p, c=c: proj_fm(p, 512 + c * 128),
                                      lambda p, c=c, sl=sl: S.op("act", lambda e: e.activation(out=srgT[:, c, sl], in_=p[:], func=AF.Silu),
                                                                 reads=[p], writes=[srgT])))
                    for tt in range(4):
                        def pj(p, tt=tt, W=W, hT_=hT_):
                            def mmv(e):
                                for k in range(8):
                                    ins = e.matmul(p[:, 0:256], lhsT=hT_[:, k, tt * 128:(tt + 1) * 128], rhs=W[:, k, 256:512],
                                                   start=(k == 0), stop=(k == 7))
                                return ins
                            S.op("pe", mmv, reads=[W, hT_], writes=[p])
                        steps.append((pj, lambda p, tt=tt, g=g: S.op("act", lambda e: e.activation(out=rv[:, g * 4 + tt, :], in_=p[:, 0:256], func=AF.Copy),
                                                                     reads=[p], writes=[rv])))
                    ps_ = [nextp() for _ in steps]
                    steps[0][0](ps_[0])
                    for i in range(len(steps)):
                        if i + 1 < len(steps):
                            steps[i + 1][0](ps_[i + 1])
                        steps[i][1](ps_[i])
                def st_x(n, h=h):
                    cs = slice(n * 128, (n + 1) * 128)
                    i2 = n % 2
                    S.op("pe", lambda e: e.matmul(pS[:], lhsT=rkT[:, cs], rhs=rqT[:, cs], start=True, stop=True),
                         reads=[rkT, rqT], writes=[pS])
                    S.op("dve", lambda e: e.tensor_tensor(out=PT2[i2][:], in0=pS[:], in1=dmask[:, h, :], op=ALU.mult),
                         reads=[pS, dmask], writes=[PT2[i2]])
                    if n < NT - 1:
                        S.op("pe", lambda e: e.transpose(pKT[:], rkT[:, cs], ident[:]), reads=[rkT, ident], writes=[pKT])
                        S.op("act", lambda e: e.activation(out=kz2[i2][:], in_=pKT[:], func=AF.Identity, scale=zeta[:, h:h + 1]),
                             reads=[pKT, zeta], writes=[kz2[i2]])

                def st_y(n, h=h):
                    cs = slice(n * 128, (n + 1) * 128)
                    i2, i3 = n % 2, n % 3
                    po_ = pA[i2]

                    def mmo(e):
                        ins = e.matmul(po_[:, 0:256], lhsT=PT2[i2][:], rhs=rv[:, n, :], start=True, stop=(n == 0))
                        if n > 0:
                            ins = e.matmul(po_[:, 0:256], lhsT=rqxT[:, cs], rhs=stbf[(n - 1) % 3][:], start=False, stop=True)
                        return ins
                    S.op("pe", mmo, reads=[PT2[i2], rv, rqxT] + ([stbf[(n - 1) % 3]] if n > 0 else []), writes=[po_])
                    if n < NT - 1:
                        S.op("pe", lambda e: e.matmul(pKV[:], lhsT=kz2[i2][:], rhs=rv[:, n, :], start=True, stop=True),
                             reads=[kz2[i2], rv], writes=[pKV])
                        if n == 0:
                            S.op("dve", lambda e: e.tensor_copy(stbf[i3][:], pKV[:]), reads=[pKV], writes=[stbf[i3]])
                            S.op("dve", lambda e: e.tensor_copy(state[:], pKV[:]), reads=[pKV], writes=[state])
                        else:
                            S.op("dve", lambda e: e.scalar_tensor_tensor(out=stbf[i3][:], in0=state[:], scalar=cdecay[h], in1=pKV[:],
                                                                         op0=ALU.mult, op1=ALU.add), reads=[state, pKV], writes=[stbf[i3]])
                            if n < NT - 2:
                                S.op("dve", lambda e: e.scalar_tensor_tensor(out=state[:], in0=state[:], scalar=cdecay[h], in1=pKV[:],
                                                                             op0=ALU.mult, op1=ALU.add), reads=[state, pKV], writes=[state])
                    oss_ = oss2[i2]
                    S.op("act", lambda e: e.activation(out=ojunk[:], in_=po_[:, 0:256], func=AF.Square, accum_out=oss_[:]),
                         reads=[po_], writes=[ojunk, oss_])
                    rstd_ops(oss_, 256, lnexp=True)
                    S.op("dve", lambda e: e.tensor_scalar(out=on2[i2][:], in0=po_[:, 0:256], scalar1=oss_[:, 0:1], scalar2=None, op0=ALU.mult),
                         reads=[po_, oss_], writes=[on2[i2]])

                def st_z(n):
                    cs = slice(n * 128, (n + 1) * 128)
                    i2 = n % 2

                    def trO(e):
                        for c in range(2):
                            ins = e.transpose(pRT[:, c, :], on2[i2][:, c * 128:(c + 1) * 128], ident[:])
                        return ins
                    S.op("pe", trO, reads=[on2[i2], ident], writes=[pRT])
                    S.op("dve", lambda e: e.tensor_tensor(out=roT[:, :, cs], in0=pRT[:], in1=srgT[:, :, cs], op=ALU.mult),
                         reads=[pRT, srgT], writes=[roT])

                st_x(0)
                st_y(0)
                for n in range(NT):
                    if n + 1 < NT:
                        st_x(n + 1)
                        st_y(n + 1)
                    st_z(n)
                S.dma("pool", roT_d[:, 2 * h:2 * h + 2, :], roT[:], roT, reads=[roT], writes=[B_roT], acc=True)
            S.barrier()
            S.emit()
        if stop <= 2:
            S.emit()
            es_w.close()
            es_rope.close()
            return nc

        with ExitStack() as ph:
            dqT = sb(ph, [128, 2, S_LEN], BF16, "dqT")
            dkT = sb(ph, [128, 2, S_LEN], BF16, "dkT")
            dv = sb(ph, [128, NT, 258], BF16, "dv")
            doT = sb(ph, [128, 2, S_LEN], BF16, "doT")
            qsb = [sb(ph, [128, 512], BF16, "qsb%d" % i) for i in range(2)]
            t1 = [sb(ph, [128, 512], F32, "t1%d" % i) for i in range(2)]
            t2 = [sb(ph, [128, 512], F32, "t2%d" % i) for i in range(2)]
            pA = [ps(ph, [128, 512], F32, "pA%d" % i) for i in range(2)]
            pswp = ps(ph, [128, 512], F32, "pswp")
            pOa = [[ps(ph, [128, 512], F32, "pO%d%d" % (qb, c)) for c in range(2)] for qb in range(2)]
            pDT = ps(ph, [128, 2, 128], BF16, "pDT")
            PTs = [sb(ph, [128, 2, 256], BF16, "PT%d" % i) for i in range(4)]
            sc_ctr = [0]
            rl = sb(ph, [128, 2], F32, "rl")
            a1 = sb(ph, [128, 256], F32, "a1")
            a2_ = sb(ph, [128, 256], F32, "a2")
            ajunk = sb(ph, [128, 256], BF16, "ajunk")
            ass_ = sb(ph, [128, 1], F32, "ass")
            don = sb(ph, [128, 256], BF16, "don")
            rt = [(qsb[i], pswp, t1[i], t2[i]) for i in range(2)]
            pai = [0]

            def nextp3():
                pai[0] += 1
                return pA[pai[0] % 2]

            osb = [sb(ph, [128, 2, 2, 257], F32, "osb%d" % i) for i in range(2)]
            pending = [None]

            dons = [[sb(ph, [128, 256], BF16, "don%d%d" % (i, j)) for j in range(2)] for i in range(2)]
            rl2 = [sb(ph, [128, 2], F32, "rl%d" % i) for i in range(2)]
            a1_2 = [sb(ph, [128, 256], F32, "a1_%d" % i) for i in range(2)]
            a2_2 = [sb(ph, [128, 256], F32, "a2_%d" % i) for i in range(2)]
            ass2 = [sb(ph, [128, 1], F32, "ass%d" % i) for i in range(2)]
            ajk2 = [sb(ph, [128, 256], BF16, "ajk%d" % i) for i in range(2)]
            micro = []

            def make_epilogue(osb_, q0, par):
                st = []
                for qb in range(2):
                    don_ = dons[par][qb]
                    rl_, a1_, a2q, ass_q, ajk = rl2[qb], a1_2[qb], a2_2[qb], ass2[qb], ajk2[qb]
                    tok = slice((q0 + qb) * 128, (q0 + qb + 1) * 128)

                    def m1(qb=qb, rl_=rl_, a1_=a1_, a2q=a2q):
                        S.op("dve", lambda e: e.reciprocal(out=rl_[:], in_=osb_[:, qb, :, 256]), reads=[osb_, rl_], writes=[rl_])
                        S.op("dve", lambda e: e.tensor_tensor(out=rl_[:, 1:2], in0=rl_[:, 1:2], in1=neglam[:], op=ALU.mult),
                             reads=[rl_, neglam], writes=[rl_])
                        S.op("dve", lambda e: e.tensor_scalar(out=a1_[:], in0=osb_[:, qb, 0, 0:256], scalar1=rl_[:, 0:1], scalar2=None,
                                                              op0=ALU.mult), reads=[osb_, rl_], writes=[a1_])
                        S.op("dve", lambda e: e.scalar_tensor_tensor(out=a2q[:], in0=osb_[:, qb, 1, 0:256], scalar=rl_[:, 1:2], in1=a1_[:],
                                                                     op0=ALU.mult, op1=ALU.add), reads=[osb_, rl_, a1_], writes=[a2q])

                    def m2(a2q=a2q, ass_q=ass_q, ajk=ajk):
                        S.op("act", lambda e: e.activation(out=ajk[:], in_=a2q[:], func=AF.Square, accum_out=ass_q[:]),
                             reads=[a2q], writes=[ajk, ass_q])

                    def m3(ass_q=ass_q):
                        S.op("dve", lambda e: e.tensor_scalar(out=ass_q[:], in0=ass_q[:], scalar1=1.0 / 256, scalar2=EPS,
                                                              op0=ALU.mult, op1=ALU.add), reads=[ass_q], writes=[ass_q])

                    def m4(ass_q=ass_q):
                        S.op("act", lambda e: e.activation(out=ass_q[:], in_=ass_q[:], func=AF.Ln), reads=[ass_q], writes=[ass_q])
                        S.op("act", lambda e: e.activation(out=ass_q[:], in_=ass_q[:], func=AF.Exp, scale=-0.5), reads=[ass_q], writes=[ass_q])

                    def m5(a2q=a2q, ass_q=ass_q, don_=don_):
                        S.op("dve", lambda e: e.scalar_tensor_tensor(out=don_[:], in0=a2q[:], scalar=ass_q[:, 0:1], in1=dnb[:],
                                                                     op0=ALU.mult, op1=ALU.mult), reads=[a2q, ass_q, dnb], writes=[don_])

                    def m6(don_=don_, tok=tok):
                        def trD(e):
                            for c in range(2):
                                ins = e.transpose(pDT[:, c, :], don_[:, c * 128:(c + 1) * 128], ident[:])
                            return ins
                        S.op("pe", trD, reads=[don_, ident], writes=[pDT])
                        S.op("dve", lambda e: e.tensor_copy(doT[:, :, tok], pDT[:]), reads=[pDT], writes=[doT])
                    st.append([m1, m2, m3, m4, m5, m6])
                out = []
                for k in range(6):
                    out.append(st[0][k])
                    out.append(st[1][k])
                return out

            S.op("pool", lambda e: e.memset(dv[:, :, 256:258], 1.0), writes=[dv])
            for h in range(4):
                W = Wh[h % 2]
                if h + 1 < 4:
                    load_cols(Wh[(h + 1) % 2], cols_diff(h + 1))
                for g in range(NG):
                    hT_ = hTg[g % 2]
                    sl = slice(g * 512, (g + 1) * 512)
                    S.dma("sp", hT_[:], hT_v[:, :, sl], hT_, reads=[B_hT], writes=[hT_])
                    cvt_issue(2)
                    steps = []
                    for (dst, c0) in [(dqT, 0), (dkT, 256)]:
                        for c in range(2):
                            def pj(p, cc=c0 + c * 128, W=W, hT_=hT_):
                                def mm(e):
                                    for k in range(8):
                                        ins = e.matmul(p[:], lhsT=W[:, k, cc:cc + 128], rhs=hT_[:, k, :], start=(k == 0), stop=(k == 7))
                                    return ins
                                S.op("pe", mm, reads=[W, hT_], writes=[p])
                            steps.append((pj, lambda p, dst=dst, c=c, g=g, sl=sl: rope_evac(rt, p, g, [(dst[:, c, sl], dst, None)])))
                    for tt in range(4):
                        def pjv(p, tt=tt, W=W, hT_=hT_):
                            def mmv(e):
                                for k in range(8):
                                    ins = e.matmul(p[:, 0:256], lhsT=hT_[:, k, tt * 128:(tt + 1) * 128], rhs=W[:, k, 512:768],
                                                   start=(k == 0), stop=(k == 7))
                                return ins
                            S.op("pe", mmv, reads=[W, hT_], writes=[p])
                        steps.append((pjv, lambda p, tt=tt, g=g: S.op("act", lambda e: e.activation(out=dv[:, g * 4 + tt, 0:256], in_=p[:, 0:256], func=AF.Copy),
                                                                      reads=[p], writes=[dv])))
                    ps_ = [nextp3() for _ in steps]
                    steps[0][0](ps_[0])
                    for i in range(len(steps)):
                        if i + 1 < len(steps):
                            steps[i + 1][0](ps_[i + 1])
                        steps[i][1](ps_[i])
                sc_scale = 128.0 ** -0.5
                for qg in range(S_LEN // 256):
                    q0 = 2 * qg
                    nkb = q0 + 2
                    items = list(range(nkb))

                    def score(jb, qg=qg, q0=q0):
                        sc_ctr[0] += 1
                        p = [pA[0], pA[1], pswp][sc_ctr[0] % 3]
                        pt = PTs[jb % 4]
                        last = (jb == q0 + 1)
                        qlo = 128 if last else 0
                        qs = slice(qg * 256 + qlo, (qg + 1) * 256)
                        ks = slice(jb * 128, (jb + 1) * 128)

                        def mm(e):
                            for c in range(2):
                                ins = e.matmul(p[:, c * 256 + qlo:(c + 1) * 256], lhsT=dkT[:, c, ks], rhs=dqT[:, c, qs],
                                               start=True, stop=True)
                            return ins
                        S.op("pe", mm, reads=[dkT, dqT], writes=[p])
                        if last:
                            for c in range(2):
                                S.op("act", lambda e, c=c: e.activation(out=pt[:, c, 128:256], in_=p[:, c * 256 + 128:(c + 1) * 256],
                                                                        func=AF.Exp, scale=sc_scale), reads=[p], writes=[pt])
                        else:
                            S.op("act", lambda e: e.activation(out=pt[:].rearrange("p c q -> p (c q)"), in_=p[:],
                                                               func=AF.Exp, scale=sc_scale), reads=[p], writes=[pt])
                        for qb in range(2):
                            if jb == q0 + qb:
                                for c in range(2):
                                    S.op("dve", lambda e, c=c, qb=qb: e.tensor_tensor(
                                        out=pt[:, c, qb * 128:(qb + 1) * 128], in0=pt[:, c, qb * 128:(qb + 1) * 128],
                                        in1=tri[:], op=ALU.mult), reads=[pt, tri], writes=[pt])

                    def pv(jb, q0=q0, nkb=nkb):
                        pt = PTs[jb % 4]
                        items_ = [(pOa[qb][c], c, qb, q0 + qb) for qb in range(2) if jb <= q0 + qb for c in range(2)]

                        def mmpv(e):
                            for (po, c, qb, lastk) in items_:
                                ins = e.matmul(po[:, 0:257], lhsT=pt[:, c, qb * 128:(qb + 1) * 128], rhs=dv[:, jb, 0:257],
                                               start=(jb == 0), stop=(jb == lastk))
                            return ins
                        S.op("pe", mmpv, reads=[pt, dv], writes=[it[0] for it in items_])
                    score(0)
                    if nkb > 1:
                        score(1)
                    for jb in items:
                        if jb + 2 < nkb:
                            score(jb + 2)
                        pv(jb)
                        if micro and jb >= 1:
                            micro.pop(0)()
                    while micro:
                        micro.pop(0)()
                    osb_ = osb[qg % 2]
                    for qb in range(2):
                        for c in range(2):
                            po = pOa[qb][c]
                            if c == 0:
                                S.op("dve", lambda e, po=po, qb=qb, c=c, osb_=osb_: e.tensor_copy(osb_[:, qb, c, :], po[:, 0:257]),
                                     reads=[po], writes=[osb_])
                            else:
                                S.op("dve", lambda e, po=po, qb=qb, c=c, osb_=osb_: e.tensor_copy(osb_[:, qb, c, :], po[:, 0:257]),
                                     reads=[po, osb_], writes=[osb_])
                    micro.extend(make_epilogue(osb_, q0, qg % 2))
                while micro:
                    micro.pop(0)()
                S.dma("pool", doT_d[:, 2 * h:2 * h + 2, :], doT[:], doT, reads=[doT], writes=[B_doT], acc=True)
            S.barrier()
            S.emit()
        if stop <= 3:
            S.emit()
            es_w.close()
            es_rope.close()
            return nc

        es_w.close()
        es_rope.close()
        cvt_issue(1000)
        with ExitStack() as ph:
            Wro = sb(ph, [128, 8, D], BF16, "Wro")
            Wdo = sb(ph, [128, 8, D], BF16, "Wdo")
            Wg = sb(ph, [128, 8, 2048], BF16, "Wg")
            S.dma("pool", Wro[:], w_ret_o.rearrange("(kc p) n -> p kc n", p=128), Wro, writes=[Wro])
            S.dma("pool", Wdo[:], w_diff_o.rearrange("(kc p) n -> p kc n", p=128), Wdo, writes=[Wdo])
            S.dma("pool", Wg[:], w_in_v[:, :, 6144:8192], Wg, writes=[Wg])
            zt = sb(ph, [128, 4, D], BF16, "zt")
            S.op("pool", lambda e: e.memset(zt[:], 0.0), writes=[zt])
            xpad_v = xpad_d.rearrange("(a p) f -> p a f", p=128)
            zf_pos = [0]

            def zero_fill(n):
                for _ in range(n):
                    a = zf_pos[0]
                    if a >= NBLK // 4:
                        return
                    zf_pos[0] += 1
                    S.dma("sp", xpad_v[:, a * 4:(a + 1) * 4, :], zt[:], zt, reads=[zt], track=[B_xz, B_xpad])
            rg_ = [sb(ph, [128, 8, 512], BF16, "rg%d" % i) for i in range(2)]
            dg_ = [sb(ph, [128, 8, 512], BF16, "dg%d" % i) for i in range(2)]
            hg_ = [sb(ph, [128, 8, 512], BF16, "hg%d" % i) for i in range(2)]
            mg_ = [sb(ph, [128, 8, 512], BF16, "mg%d" % i) for i in range(2)]
            pp = [ps(ph, [128, 512], F32, "pp%d" % i) for i in range(8)]
            sg = [sb(ph, [128, 512], BF16, "sg%d" % i) for i in range(4)]
            m1 = [sb(ph, [128, 512], F32, "m1%d" % i) for i in range(2)]
            m2 = [sb(ph, [128, 512], F32, "m2%d" % i) for i in range(2)]
            it = 0
            for g in range(NG):
                sl = slice(g * 512, (g + 1) * 512)
                r_, d_, h_, m_ = rg_[g % 2], dg_[g % 2], hg_[g % 2], mg_[g % 2]
                S.dma("sp", r_[:], roT_d[:, :, sl], r_, reads=[B_roT], writes=[r_])
                S.dma("sp", d_[:], doT_d[:, :, sl], d_, reads=[B_doT], writes=[d_])
                S.dma("sp", h_[:], hT_v[:, :, sl], h_, reads=[B_hT], writes=[h_])
                zero_fill((NBLK // 4 + NG - 1) // NG)
                for n in range(8):
                    P4 = pp[(it % 2) * 4:(it % 2) * 4 + 4]
                    sgr, sgd = sg[(it % 2) * 2], sg[(it % 2) * 2 + 1]
                    m1_, m2_ = m1[it % 2], m2[it % 2]
                    it += 1
                    ns = slice(n * 128, (n + 1) * 128)
                    for (p, Wt, src, c0) in [(P4[0], Wro, r_, 0), (P4[1], Wdo, d_, 0), (P4[2], Wg, h_, 0), (P4[3], Wg, h_, 1024)]:
                        def mm(e, p=p, Wt=Wt, src=src, c0=c0, n=n):
                            for k in range(8):
                                ins = e.matmul(p[:], lhsT=Wt[:, k, c0 + n * 128:c0 + (n + 1) * 128], rhs=src[:, k, :],
                                               start=(k == 0), stop=(k == 7))
                            return ins
                        S.op("pe", mm, reads=[Wt, src], writes=[p])
                    S.op("act", lambda e, p=P4[2], o=sgr: e.activation(out=o[:], in_=p[:], func=AF.Sigmoid), reads=[P4[2]], writes=[sgr])
                    S.op("act", lambda e, p=P4[3], o=sgd: e.activation(out=o[:], in_=p[:], func=AF.Sigmoid), reads=[P4[3]], writes=[sgd])
                    S.op("dve", lambda e, p=P4[0], o=m1_, s_=sgr: e.tensor_tensor(out=o[:], in0=p[:], in1=s_[:], op=ALU.mult),
                         reads=[P4[0], sgr], writes=[m1_])
                    S.op("dve", lambda e, p=P4[1], o=m2_, s_=sgd: e.tensor_tensor(out=o[:], in0=p[:], in1=s_[:], op=ALU.mult),
                         reads=[P4[1], sgd], writes=[m2_])
                    S.op("pool", lambda e, a=m1_, b=m2_, m_=m_, n=n: e.tensor_tensor(out=m_[:, n, :], in0=a[:], in1=b[:], op=ALU.add),
                         reads=[m1_, m2_], writes=[m_])
                S.dma("pool", mT_d[:, :, sl], m_[:], m_, reads=[m_], writes=[B_mT], acc=True)
            S.barrier()
            S.emit()
        if stop <= 4:
            S.emit()
            return nc

        with ExitStack() as ph:
            Wo = sb(ph, [128, 8, D], BF16, "Wo")
            S.dma("pool", Wo[:], w_out.rearrange("(kc p) n -> p kc n", p=128), Wo, writes=[Wo])
            for k in range(8):
                S.op("dve", lambda e, k=k: e.tensor_tensor(out=Wo[:, k, :], in0=Wo[:, k, :], in1=g1b[:], op=ALU.mult),
                     reads=[Wo], writes=[Wo])
            h2all = sb(ph, [128, NT, D], BF16, "h2all")
            h2b = [Buf("h2all%d" % i) for i in range(NT)]
            lgall = sb(ph, [128, NT, 36], F32, "lgall")
            lgb = [Buf("lg%d" % i) for i in range(NT)]
            ph2 = ExitStack()
            mg_ = [sb(ph2, [128, 8, 512], BF16, "mg%d" % i) for i in range(2)]
            xt = [sb(ph2, [128, D], F32, "xt%d" % i) for i in range(2)]
            x1t = [sb(ph2, [128, D], F32, "x1t%d" % i) for i in range(2)]
            xn2 = [sb(ph2, [128, D], F32, "xn2%d" % i) for i in range(2)]
            junk = sb(ph2, [128, D], BF16, "junk")
            ssq = [sb(ph2, [128, 1], F32, "ss%d" % i) for i in range(2)]
            h2f = [sb(ph2, [128, 8, 128], F32, "h2f%d" % i) for i in range(2)]
            po = [ps(ph2, [128, 512], F32, "po%d" % i) for i in range(4)]
            pT2 = [ps(ph2, [128, 4, 128], F32, "pT2%d" % i) for i in range(2)]
            pR = [ps(ph2, [128, 36], F32, "pR%d" % i) for i in range(2)]
            def p4_a1(t):
                g = t // 4
                tt = t % 4
                m_ = mg_[g % 2]
                if tt == 0:
                    S.dma("sp", m_[:], mT_d[:, :, g * 512:(g + 1) * 512], m_, reads=[B_mT], writes=[m_])
                x_ = xt[t % 2]
                x1_ = x1t[t % 2]
                S.dma("sp", x_[:], x_d[t * 128:(t + 1) * 128, :], x_, writes=[x_])
                for half in range(2):
                    p = po[(t % 2) * 2 + half]
                    hs = slice(half * 512, (half + 1) * 512)

                    def mm(e, p=p, hs=hs):
                        for k in range(8):
                            ins = e.matmul(p[:], lhsT=m_[:, k, tt * 128:(tt + 1) * 128], rhs=Wo[:, k, hs], start=(k == 0), stop=(k == 7))
                        return ins
                    S.op("pe", mm, reads=[m_, Wo], writes=[p])
                    S.op("dve", lambda e, p=p, hs=hs: e.tensor_tensor(out=x1_[:, hs], in0=p[:], in1=x_[:, hs], op=ALU.add),
                         reads=[p, x_], writes=[x1_])
                S.dma("sp", x1_d[t * 128:(t + 1) * 128, :], x1_[:], x1_, reads=[x1_], track=[B_x1])

            def p4_a2(t):
                x1_ = x1t[t % 2]
                xn_ = xn2[t % 2]
                ss = ssq[t % 2]
                S.op("act", lambda e: e.activation(out=junk[:], in_=x1_[:], func=AF.Square, accum_out=ss[:]),
                     reads=[x1_], writes=[junk, ss])
                rstd_ops(ss, D, lnexp=True)
                S.op("dve", lambda e: e.tensor_scalar(out=xn_[:], in0=x1_[:], scalar1=ss[:, 0:1], scalar2=None, op0=ALU.mult),
                     reads=[x1_, ss], writes=[xn_])

            def p4_b1(t):
                x1_ = x1t[t % 2]
                xn_ = xn2[t % 2]
                ss = ssq[t % 2]
                hf = h2f[t % 2]
                for hh in range(2):
                    p = pT2[hh]

                    def tr(e, p=p, hh=hh):
                        for c in range(4):
                            cc = hh * 4 + c
                            ins = e.transpose(p[:, c, :], xn_[:, cc * 128:(cc + 1) * 128], identf[:])
                        return ins
                    S.op("pe", tr, reads=[xn_, identf], writes=[p])
                    for c in range(4):
                        cc = hh * 4 + c
                        if c % 2 == 0:
                            S.op("act", lambda e, p=p, c=c, cc=cc: e.activation(
                                out=hf[:, cc, :], in_=p[:, c, :], func=AF.Identity, bias=modT[:, 24 + cc:25 + cc],
                                scale=A2[:, cc:cc + 1]), reads=[p], writes=[hf])
                        else:
                            S.op("dve", lambda e, p=p, c=c, cc=cc: e.tensor_scalar(
                                out=hf[:, cc, :], in0=p[:, c, :], scalar1=A2[:, cc:cc + 1], scalar2=modT[:, 24 + cc:25 + cc],
                                op0=ALU.mult, op1=ALU.add), reads=[p], writes=[hf])
                S.op("pool", lambda e: e.tensor_tensor(out=x1_[:], in0=xn_[:], in1=A2b[:], op=ALU.mult), reads=[xn_, x1_], writes=[x1_])
                S.op("pool", lambda e: e.tensor_tensor(out=h2all[:, t, :], in0=x1_[:], in1=B2b[:], op=ALU.add),
                     reads=[x1_], writes=[h2b[t]])

            def p4_b2(t):
                hf = h2f[t % 2]
                pr_ = pR[t % 2]

                def mmr(e):
                    for k in range(8):
                        ins = e.matmul(pr_[:], lhsT=hf[:, k, :], rhs=wrt[:, k, :], start=(k == 0), stop=(k == 7))
                    return ins
                S.op("pe", mmr, reads=[hf, wrt], writes=[pr_])
                S.op("dve", lambda e: e.tensor_tensor(out=lgall[:, t, :], in0=pr_[:], in1=rbb[:], op=ALU.add),
                     reads=[pr_], writes=[lgb[t]])

            p4_a1(0)
            p4_a2(0)
            for t in range(NT):
                if t + 1 < NT:
                    p4_a1(t + 1)
                p4_b1(t)
                if t + 1 < NT:
                    p4_a2(t + 1)
                if t >= 1:
                    p4_b2(t - 1)
            p4_b2(NT - 1)
            S.barrier()
            S.emit()
            ph2.close()
            V = "dve"
            gl = lgall[:, :, 0:4]
            el4 = lgall[:, :, 4:36].rearrange("p t (g j) -> p t g j", g=4)
            gmax = sb(ph, [128, NT], F32, "gmax")
            maskg = sb(ph, [128, NT, 4], F32, "maskg")
            gsh = sb(ph, [128, NT, 4], F32, "gsh")
            gsum = sb(ph, [128, NT], F32, "gsum")
            sel = sb(ph, [128, NT, 4, 8], F32, "sel")
            els = sb(ph, [128, NT, 8], F32, "els")
            el2 = sb(ph, [128, NT, 8], F32, "el2")
            mk = [sb(ph, [128, NT, 8], F32, "mk%d" % i) for i in range(2)]
            m12 = [sb(ph, [128, NT], F32, "m12_%d" % i) for i in range(2)]
            ee = sb(ph, [128, NT], F32, "ee")
            den = sb(ph, [128, NT], F32, "den")

            def bc(ap2, n):
                return ap2.unsqueeze(2).to_broadcast([128, NT, n])
            S.op(V, lambda e: e.reduce_max(out=gmax[:], in_=gl, axis=AX.X), reads=lgb, writes=[gmax])
            S.op(V, lambda e: e.tensor_tensor(out=maskg[:], in0=gl, in1=bc(gmax[:], 4), op=ALU.is_equal), reads=lgb + [gmax], writes=[maskg])
            S.op(V, lambda e: e.tensor_tensor(out=gsh[:], in0=gl, in1=bc(gmax[:], 4), op=ALU.subtract), reads=lgb + [gmax], writes=[gsh])
            S.op("act", lambda e: e.activation(out=gsh[:], in_=gsh[:], func=AF.Exp), reads=[gsh], writes=[gsh])
            S.op(V, lambda e: e.reduce_sum(out=gsum[:], in_=gsh[:], axis=AX.X), reads=[gsh], writes=[gsum])
            S.op(V, lambda e: e.reciprocal(out=gsum[:], in_=gsum[:]), reads=[gsum], writes=[gsum])
            S.op(V, lambda e: e.tensor_tensor(out=sel[:], in0=el4, in1=maskg[:].unsqueeze(3).to_broadcast([128, NT, 4, 8]), op=ALU.mult),
                 reads=lgb + [maskg], writes=[sel])
            S.op(V, lambda e: e.tensor_tensor(out=els[:], in0=sel[:, :, 0, :], in1=sel[:, :, 1, :], op=ALU.add), reads=[sel], writes=[els])
            S.op(V, lambda e: e.tensor_tensor(out=els[:], in0=els[:], in1=sel[:, :, 2, :], op=ALU.add), reads=[sel, els], writes=[els])
            S.op(V, lambda e: e.tensor_tensor(out=els[:], in0=els[:], in1=sel[:, :, 3, :], op=ALU.add), reads=[sel, els], writes=[els])
            S.op(V, lambda e: e.reduce_max(out=m12[0][:], in_=els[:], axis=AX.X), reads=[els], writes=[m12[0]])
            S.op(V, lambda e: e.tensor_tensor(out=mk[0][:], in0=els[:], in1=bc(m12[0][:], 8), op=ALU.is_equal), reads=[els, m12[0]], writes=[mk[0]])
            S.op(V, lambda e: e.scalar_tensor_tensor(out=el2[:], in0=mk[0][:], scalar=-1e30, in1=els[:], op0=ALU.mult, op1=ALU.add),
                 reads=[mk[0], els], writes=[el2])
            S.op(V, lambda e: e.reduce_max(out=m12[1][:], in_=el2[:], axis=AX.X), reads=[el2], writes=[m12[1]])
            S.op(V, lambda e: e.tensor_tensor(out=mk[1][:], in0=el2[:], in1=bc(m12[1][:], 8), op=ALU.is_equal), reads=[el2, m12[1]], writes=[mk[1]])
            S.op(V, lambda e: e.tensor_tensor(out=ee[:], in0=m12[1][:], in1=m12[0][:], op=ALU.subtract), reads=m12, writes=[ee])
            S.op("act", lambda e: e.activation(out=ee[:], in_=ee[:], func=AF.Exp), reads=[ee], writes=[ee])
            S.op(V, lambda e: e.tensor_scalar(out=den[:], in0=ee[:], scalar1=1.0, scalar2=None, op0=ALU.add), reads=[ee], writes=[den])
            S.op(V, lambda e: e.reciprocal(out=den[:], in_=den[:]), reads=[den], writes=[den])
            S.op(V, lambda e: e.tensor_tensor(out=wAB[:, :, 0], in0=den[:], in1=gsum[:], op=ALU.mult), reads=[den, gsum], writes=[wAB])
            S.op(V, lambda e: e.tensor_tensor(out=wAB[:, :, 1], in0=wAB[:, :, 0], in1=ee[:], op=ALU.mult), reads=[ee, wAB], writes=[wAB])
            for k in range(2):
                S.op(V, lambda e, k=k: e.tensor_tensor(
                    out=MM[:, :, k, :].rearrange("p t (g j) -> p t g j", g=4),
                    in0=maskg[:].unsqueeze(3).to_broadcast([128, NT, 4, 8]),
                    in1=mk[k][:].unsqueeze(2).to_broadcast([128, NT, 4, 8]), op=ALU.mult),
                    reads=[maskg, mk[k], MM], writes=[MM])
            rank = sb(ph, [128, NT, 32], F32, "rank")
            msall = sb(ph, [128, NT, 32], F32, "msall")
            csa = sb(ph, [128, NT, 32], F32, "csa")
            csb = sb(ph, [128, NT, 32], F32, "csb")
            pcs = sb(ph, [128, NT, 32], F32, "pcs")
            base = sb(ph, [128, 32], F32, "base")
            ppr = [ps(ph, [128, 512], F32, "ppr%d" % i) for i in range(2)]
            ppc = [ps(ph, [128, 512], F32, "ppc%d" % i) for i in range(2)]
            S.op("dve", lambda e: e.tensor_tensor(out=msall[:], in0=MM[:, :, 0, :], in1=MM[:, :, 1, :], op=ALU.add), reads=[MM], writes=[msall])
            msf = msall[:].rearrange("p t e -> p (t e)")
            for hh in range(2):
                S.op("pe", lambda e, hh=hh: e.matmul(ppr[hh][:], lhsT=ltri[:], rhs=msf[:, hh * 512:(hh + 1) * 512], start=True, stop=True),
                     reads=[msall, ltri], writes=[ppr[hh]])
                S.op("pe", lambda e, hh=hh: e.matmul(ppc[hh][:], lhsT=onesf[:], rhs=msf[:, hh * 512:(hh + 1) * 512], start=True, stop=True),
                     reads=[msall, onesf], writes=[ppc[hh]])
                S.op("dve", lambda e, hh=hh: e.tensor_copy(pcs[:, hh * 16:(hh + 1) * 16, :].rearrange("p t e -> p (t e)"), ppc[hh][:]),
                     reads=[ppc[hh]], writes=[pcs])
            src, dst = pcs, csa
            sh = 1
            first = True
            while sh < NT:
                S.op("dve", lambda e, src=src, dst=dst, sh=sh: e.tensor_copy(dst[:, 0:sh, :], src[:, 0:sh, :]), reads=[src, dst], writes=[dst])
                S.op("dve", lambda e, src=src, dst=dst, sh=sh: e.tensor_tensor(out=dst[:, sh:NT, :], in0=src[:, sh:NT, :], in1=src[:, 0:NT - sh, :], op=ALU.add),
                     reads=[src, dst], writes=[dst])
                src, dst = dst, (csb if dst is csa else csa)
                sh *= 2
            incl = src
            S.op("dve", lambda e: e.tensor_copy(base[:], incl[:, NT - 1, :]), reads=[incl], writes=[base])
            S.op("dve", lambda e: e.tensor_tensor(out=dst[:], in0=incl[:], in1=pcs[:], op=ALU.subtract), reads=[incl, pcs, dst], writes=[dst])
            for hh in range(2):
                S.op("dve", lambda e, hh=hh, dst=dst: e.tensor_tensor(out=rank[:, hh * 16:(hh + 1) * 16, :].rearrange("p t e -> p (t e)"),
                                                                      in0=ppr[hh][:], in1=dst[:, hh * 16:(hh + 1) * 16, :].rearrange("p t e -> p (t e)"),
                                                                      op=ALU.add), reads=[ppr[hh], dst, rank], writes=[rank])
            nbk = sb(ph, [128, 32], F32, "nbk")
            psb = sb(ph, [128, 33], F32, "psb")
            pstart = sb(ph, [128, 32], F32, "pstart")
            S.op("dve", lambda e: e.tensor_scalar(out=nbk[:], in0=base[:], scalar1=1.0 / RB,
                                                  scalar2=((RB - 1.0) / RB - 0.5 + 0.5 / RB), op0=ALU.mult, op1=ALU.add),
                 reads=[base], writes=[nbk])
            S.op("dve", lambda e: e.tensor_scalar(out=nbk[:], in0=nbk[:], scalar1=MAGIC, scalar2=None, op0=ALU.add),
                 reads=[nbk], writes=[nbk])
            S.op("dve", lambda e: e.tensor_scalar(out=nbk[:], in0=nbk[:], scalar1=-MAGIC, scalar2=None, op0=ALU.add),
                 reads=[nbk], writes=[nbk])
            S.op("dve", lambda e: e.memset(psb[:, 0:1], 0.0), writes=[psb])
            for ex in range(32):
                S.op("dve", lambda e, ex=ex: e.tensor_tensor(out=psb[:, ex + 1:ex + 2], in0=psb[:, ex:ex + 1], in1=nbk[:, ex:ex + 1], op=ALU.add),
                     reads=[psb, nbk], writes=[psb])
            S.op("dve", lambda e: e.tensor_scalar(out=pstart[:], in0=psb[:, 0:32], scalar1=float(RB), scalar2=None, op0=ALU.mult),
                 reads=[psb], writes=[pstart])
            blk_e = sb(ph, [128, NBIG], F32, "blk_e")
            cmp3 = sb(ph, [128, NBIG, 32], F32, "cmp3")
            S.op("dve", lambda e: e.tensor_tensor(out=cmp3[:], in0=psb[:, 1:33].unsqueeze(1).to_broadcast([128, NBIG, 32]),
                                                  in1=iota_b[:, 0:NBIG].unsqueeze(2).to_broadcast([128, NBIG, 32]), op=ALU.is_le),
                 reads=[psb, iota_b], writes=[cmp3])
            S.op("dve", lambda e: e.reduce_sum(out=blk_e[:], in_=cmp3[:], axis=AX.X), reads=[cmp3], writes=[blk_e])
            S.op("dve", lambda e: e.tensor_scalar(out=blk_e[:], in0=blk_e[:], scalar1=31.0, scalar2=None, op0=ALU.min),
                 reads=[blk_e], writes=[blk_e])
            rr = sb(ph, [128, NT, 32], F32, "rr")
            prod = sb(ph, [128, NT, 2, 32], F32, "prod")
            destf = sb(ph, [128, NT, 2], F32, "destf")
            S.op("dve", lambda e: e.tensor_tensor(out=rr[:], in0=rank[:], in1=pstart[:].unsqueeze(1).to_broadcast([128, NT, 32]), op=ALU.add),
                 reads=[rank, pstart], writes=[rr])
            for k in range(2):
                S.op("dve", lambda e, k=k: e.tensor_tensor(out=prod[:, :, k, :], in0=rr[:], in1=MM[:, :, k, :], op=ALU.mult),
                     reads=[rr, MM, prod], writes=[prod])
            S.op("dve", lambda e: e.reduce_sum(out=destf[:], in_=prod[:], axis=AX.X), reads=[prod], writes=[destf])
            S.op("dve", lambda e: e.tensor_copy(desti[:], destf[:]), reads=[destf], writes=[desti])
            wf1 = sb(ph, [128, NBIG], F32, "wf1")
            S.op("dve", lambda e: e.tensor_scalar(out=wf1[:], in0=blk_e[:], scalar1=128.0, scalar2=vec[:, 2:3], op0=ALU.mult, op1=ALU.add),
                 reads=[blk_e, vec], writes=[wf1])
            S.op("dve", lambda e: e.tensor_copy(widx1[:], wf1[:]), reads=[wf1], writes=[widx1])
            for t in range(NT):
                def scat(e, sem, t=t):
                    for k in range(2):
                        e.indirect_dma_start(out=xpad_d[:, :], out_offset=bass.IndirectOffsetOnAxis(ap=desti[:, t, k:k + 1], axis=0),
                                             in_=h2all[:, t, :], in_offset=None).then_inc(sem, 16)
                S.custom("pool", scat, h2all, 2, reads=[h2b[t], desti, B_xz], track=[B_xpad])
            if debug:
                dbg_d = nc.dram_tensor("dbg_d", [128, NBIG + 64 + 64], F32, kind="ExternalOutput").ap()
                dbt = sb(ph, [128, NBIG + 128], F32, "dbt")
                S.op("dve", lambda e: e.tensor_copy(dbt[:, 0:NBIG], blk_e[:]), reads=[blk_e], writes=[dbt])
                S.op("dve", lambda e: e.tensor_copy(dbt[:, NBIG:NBIG + 64], destf[:].rearrange("p t k -> p (t k)")), reads=[destf, dbt], writes=[dbt])
                S.op("dve", lambda e: e.tensor_copy(dbt[:, NBIG + 64:NBIG + 128], wAB[:].rearrange("p t k -> p (t k)")), reads=[wAB, dbt], writes=[dbt])
                S.dma("sp", dbg_d, dbt[:], dbt, reads=[dbt], writes=[Buf("dbg")])
            S.barrier()
            S.emit()
        if stop <= 5:
            S.emit()
            return nc

        with ExitStack() as ph:
            NW = 3 if NSUB == 1 else 2
            W1 = [sb(ph, [128, 8 * D_EXP], BF16, "W1_%d" % i) for i in range(NW)]
            W3 = [sb(ph, [128, 8 * D_EXP], BF16, "W3_%d" % i) for i in range(NW)]
            W2 = [sb(ph, [128, 4 * D], BF16, "W2_%d" % i) for i in range(NW)]
            pXT = [ps(ph, [128, 8, 128], BF16, "pXT%d" % i) for i in range(1)]
            pG = [ps(ph, [128, 512], F32, "pG%d" % i) for i in range(2)]
            pU = [ps(ph, [128, 512], F32, "pU%d" % i) for i in range(2)]
            pAT = [ps(ph, [128, 4, 128], BF16, "pAT%d" % i) for i in range(1)]
            pY = [ps(ph, [128, 512], F32, "pY%d" % i) for i in range(2)]
            sgt = [sb(ph, [128, 512], BF16, "sgt%d" % i) for i in range(2)]
            actt = [sb(ph, [128, 512], BF16, "act%d" % i) for i in range(2)]
            actT = [sb(ph, [128, 4, 128], BF16, "actT%d" % i) for i in range(2)]
            yblk = [sb(ph, [128, D], F32, "yblk%d" % i) for i in range(2)]

            def load_w(bb):
                b = bb
                wi = bb % NW
                for (Wt, rows) in [(W1[wi], wb_eg), (W3[wi], wb_eu), (W2[wi], wb_ed)]:
                    def g1(e, sem, Wt=Wt, rows=rows, b=b):
                        e.indirect_dma_start(out=Wt[:, :], out_offset=None, in_=rows[:, :],
                                             in_offset=bass.IndirectOffsetOnAxis(ap=widx1[:, b:b + 1], axis=0)).then_inc(sem, 16)
                    S.custom("pool", g1, Wt, 1, reads=[widx1, B_wb], writes=[Wt])

            xb4 = [sb(ph, [128, D], BF16, "xb4_%d" % i) for i in range(4)]
            xT3 = [sb(ph, [128, 8, 128], BF16, "xT3_%d" % i) for i in range(3)]

            def stage_x(b):
                x_ = xb4[b % 4]
                S.dma("sp", x_[:], xpad_d[b * 128:(b + 1) * 128, :], x_, reads=[B_xpad], writes=[x_])

            def stage_a(b):
                if b % NSUB == 0:
                    load_w(b // NSUB)
                x_ = xb4[b % 4]
                xT = xT3[b % 3]
                pxt = pXT[0]

                def trx(e):
                    for c in range(8):
                        ins = e.transpose(pxt[:, c, :], x_[:, c:D:8], ident[:])
                    return ins
                S.op("pe", trx, reads=[x_, ident], writes=[pxt])
                S.op("dve", lambda e: e.tensor_copy(xT[:], pxt[:]), reads=[pxt], writes=[xT])

            def stage_b(b):
                wi = (b // NSUB) % NW
                xT = xT3[b % 3]
                pg, pu = pG[b % 2], pU[b % 2]
                for (p, Wt) in [(pg, W1[wi]), (pu, W3[wi])]:
                    def mm(e, p=p, Wt=Wt):
                        for k in range(8):
                            ins = e.matmul(p[:], lhsT=xT[:, k, :], rhs=Wt[:, k * D_EXP:(k + 1) * D_EXP], start=(k == 0), stop=(k == 7))
                        return ins
                    S.op("pe", mm, reads=[xT, Wt], writes=[p])
                s_, a_ = sgt[b % 2], actt[b % 2]
                S.op("act", lambda e: e.activation(out=s_[:], in_=pg[:], func=AF.Silu), reads=[pg], writes=[s_])
                S.op("dve", lambda e: e.tensor_tensor(out=a_[:], in0=pu[:], in1=s_[:], op=ALU.mult), reads=[pu, s_], writes=[a_])

            def stage_c(b):
                wi = (b // NSUB) % NW
                a_ = actt[b % 2]
                pat = pAT[0]
                aT = actT[b % 2]
                yb2 = yblk[b % 2]

                def tr(e):
                    for c in range(4):
                        ins = e.transpose(pat[:, c, :], a_[:, c:D_EXP:4], ident[:])
                    return ins
                S.op("pe", tr, reads=[a_, ident], writes=[pat])
                S.op("act", lambda e: e.activation(out=aT[:], in_=pat[:], func=AF.Copy), reads=[pat], writes=[aT])
                for half in range(2):
                    py = pY[half]
                    hs = slice(half * 512, (half + 1) * 512)

                    def mm(e, py=py, hs=hs):
                        for k in range(4):
                            ins = e.matmul(py[:], lhsT=aT[:, k, :], rhs=W2[wi][:, k * D + hs.start:k * D + hs.stop], start=(k == 0), stop=(k == 3))
                        return ins
                    S.op("pe", mm, reads=[aT, W2[wi]], writes=[py])
                    if half == 0:
                        S.op("dve", lambda e, py=py, hs=hs: e.tensor_copy(yb2[:, hs], py[:]), reads=[py], writes=[yb2])
                    else:
                        S.op("act", lambda e, py=py, hs=hs: e.activation(out=yb2[:, hs], in_=py[:], func=AF.Copy), reads=[py, yb2], writes=[yb2])
                S.dma("act", ypad_d[b * 128:(b + 1) * 128, :], yb2[:], yb2, reads=[yb2], writes=[B_ypad], acc=True)

            for b in range(3):
                stage_x(b)
            stage_a(0)
            stage_a(1)
            stage_b(0)
            for b in range(NBLK):
                if b + 3 < NBLK:
                    stage_x(b + 3)
                if b + 2 < NBLK:
                    stage_a(b + 2)
                if b + 1 < NBLK:
                    stage_b(b + 1)
                stage_c(b)
            S.barrier()
            S.emit()

        with ExitStack() as ph:
            NB_F = 4
            y0t = [sb(ph, [128, D], F32, "y0t%d" % i) for i in range(NB_F)]
            y1t = [sb(ph, [128, D], F32, "y1t%d" % i) for i in range(NB_F)]
            x1t = [sb(ph, [128, D], F32, "x1t%d" % i) for i in range(NB_F)]
            ot = [sb(ph, [128, D], F32, "ot%d" % i) for i in range(NB_F)]
            junk = sb(ph, [128, D], BF16, "junk")
            ssq = [sb(ph, [128, 1], F32, "ss%d" % i) for i in range(NB_F)]

            def fin_load(t):
                x1_ = x1t[t % NB_F]
                ya, yb3 = y0t[t % NB_F], y1t[t % NB_F]
                for k, yt in enumerate([ya, yb3]):
                    def gath(e, sem, yt=yt, t=t, k=k):
                        e.indirect_dma_start(out=yt[:, :], out_offset=None, in_=ypad_d[:, :],
                                             in_offset=bass.IndirectOffsetOnAxis(ap=desti[:, t, k:k + 1], axis=0)).then_inc(sem, 16)
                    S.custom("pool", gath, yt, 1, reads=[desti, B_ypad], writes=[yt])
                S.dma("sp", x1_[:], x1_d[t * 128:(t + 1) * 128, :], x1_, reads=[B_x1], writes=[x1_])

            def fin_a(t):
                o_ = ot[t % NB_F]
                ya, yb3 = y0t[t % NB_F], y1t[t % NB_F]
                S.op("dve", lambda e: e.tensor_scalar(out=o_[:], in0=ya[:], scalar1=wAB[:, t, 0:1], scalar2=None, op0=ALU.mult),
                     reads=[ya, wAB], writes=[o_])
                S.op("dve", lambda e: e.scalar_tensor_tensor(out=o_[:], in0=yb3[:], scalar=wAB[:, t, 1:2], in1=o_[:],
                                                             op0=ALU.mult, op1=ALU.add), reads=[yb3, wAB, o_], writes=[o_])
                S.op("pool", lambda e: e.tensor_tensor(out=o_[:], in0=o_[:], in1=g2b[:], op=ALU.mult), reads=[o_], writes=[o_])

            def fin_b(t):
                x1_ = x1t[t % NB_F]
                o_ = ot[t % NB_F]
                ss = ssq[t % NB_F]
                S.op("dve", lambda e: e.tensor_tensor(out=o_[:], in0=o_[:], in1=x1_[:], op=ALU.add), reads=[o_, x1_], writes=[o_])
                S.op("act", lambda e: e.activation(out=junk[:], in_=o_[:], func=AF.Square, accum_out=ss[:]),
                     reads=[o_], writes=[junk, ss])
                rstd_ops(ss, D)
                S.op("dve", lambda e: e.scalar_tensor_tensor(out=o_[:], in0=o_[:], scalar=ss[:, 0:1], in1=nfb[:],
                                                             op0=ALU.mult, op1=ALU.mult), reads=[o_, ss], writes=[o_])
                S.dma("act", out_d[t * 128:(t + 1) * 128, :], o_[:], o_, reads=[o_], writes=[B_out], acc=True)

            for t in range(min(NB_F - 1, NT)):
                fin_load(t)
            fin_a(0)
            for t in range(NT):
                if t + NB_F - 1 < NT:
                    fin_load(t + NB_F - 1)
                if t + 1 < NT:
                    fin_a(t + 1)
                fin_b(t)
            S.barrier()
        S.emit()
    return nc


def make_in_maps(inputs):
    f = lambda k: np.ascontiguousarray(np.asarray(inputs[k]))
    x = f("x")
    c = f("c")
    pos = f("positions").astype(np.int32)
    aux = np.concatenate([f("b_ada")[0], f("norm_mix")[0], f("norm_ffn")[0], f("norm_final"), f("diff_norm")[0],
                          f("b_router_group")[0], f("b_router_expert")[0]]).astype(np.float32)[None, :]
    lam = np.stack([f("lam_q1")[0], f("lam_k1")[0], f("lam_q2")[0], f("lam_k2")[0]]).astype(np.float32)
    w_rt = np.ascontiguousarray(np.concatenate([f("w_router_group")[0], f("w_router_expert")[0]], axis=1))
    shared = {
        "w_ada": f("w_ada")[0], "aux": aux, "w_in": f("w_in")[0], "w_ret_o": f("w_ret_o")[0], "w_diff_o": f("w_diff_o")[0],
        "w_out": f("w_out")[0], "lam": lam, "w_rt": w_rt, "w_exp_gate": f("w_exp_gate")[0].reshape(N_EXP * 128, 8 * D_EXP), "w_exp_up": f("w_exp_up")[0].reshape(N_EXP * 128, 8 * D_EXP),
        "w_exp_down": f("w_exp_down")[0].reshape(N_EXP * 128, 4 * D),
    }
    for k, v in CONSTS.items():
        if not k.startswith("_"):
            shared[k] = v
    maps = []
    for b in range(x.shape[0]):
        m = dict(shared)
        m["x"] = np.ascontiguousarray(x[b])
        m["c"] = np.ascontiguousarray(c[b].reshape(8, 128).T)
        m["positions"] = np.ascontiguousarray(pos[b][None, :])
        maps.append(m)
    return maps


_NC_CACHE = {}


def kernel(**inputs):
    maps = make_in_maps(inputs)
    if "nc" not in _NC_CACHE:
        _NC_CACHE["nc"] = build()
    nc = _NC_CACHE["nc"]
    res = run_bass_kernel_spmd(nc, maps, core_ids=list(range(8)))
    out = np.stack([np.asarray(r["out"]) for r in res.results], axis=0)
    return out.astype(np.float32)
```

```python
import math
from contextlib import ExitStack

import numpy as np
import ml_dtypes
import concourse.bass as bass
import concourse.mybir as mybir
from concourse.bass_utils import run_bass_kernel_spmd

F32 = mybir.dt.float32
BF16 = mybir.dt.bfloat16
I32 = mybir.dt.int32
AF = mybir.ActivationFunctionType
ALU = mybir.AluOpType
AX = mybir.AxisListType

S_LEN = 4096
D = 1024
NT = 32
NG = 8
EPS = 1e-6
LAMBDA_INIT = 0.8 - 0.6 * math.exp(0.0)
MAGIC = 12582912.0
N_EXP = 32
D_EXP = 512


class Buf:
    def __init__(self, name=""):
        self.name = name
        self.w = []
        self.r = []
        self.dsem = None


class TB:
    def __init__(self, t, name):
        self.t = t
        self.b = Buf(name)

    def __getitem__(self, k):
        return self.t[k]


class Sched:
    def __init__(self, nc, es):
        self.nc = nc
        self.es = es
        self.engs = ["pe", "act", "dve", "pool", "sp"]
        self.prog = {k: [] for k in self.engs}
        self.sems = {}
        self.cnt = {}
        self.waited = {k: {} for k in self.engs}
        for k in self.engs:
            self._mksem("E_" + k)
        self.nd = 0

    def _mksem(self, name):
        s = self.es.enter_context(self.nc.semaphore(name))
        self.sems[name] = s
        self.cnt[name] = 0
        return name

    def _deps(self, eng, reads, writes):
        deps = {}
        for b in reads:
            for (s, v) in b.w:
                deps[s] = max(deps.get(s, 0), v)
        for b in writes:
            for (s, v) in b.w + b.r:
                deps[s] = max(deps.get(s, 0), v)
        out = []
        for s, v in deps.items():
            if eng == "pe" and s == "E_pe":
                continue
            if self.waited[eng].get(s, 0) >= v:
                continue
            self.waited[eng][s] = v
            out.append((s, v))
        return out

    def _commit(self, tok, reads, writes, acc=False):
        for b in reads:
            b.r.append(tok)
            if len(b.r) > 64:
                m = {}
                for (s, v) in b.r:
                    m[s] = max(m.get(s, 0), v)
                b.r = list(m.items())
        for b in writes:
            if acc:
                b.w.append(tok)
            else:
                b.w = [tok]
            b.r = []

    @staticmethod
    def _bl(xs):
        return [x.b if isinstance(x, TB) else x for x in xs]

    def op(self, eng, fn, reads=(), writes=()):
        reads = self._bl(reads)
        writes = self._bl(writes)
        waits = self._deps(eng, reads, writes)
        sname = "E_" + eng
        self.cnt[sname] += 1
        val = self.cnt[sname]
        sems = self.sems

        def run(e, waits=waits, fn=fn, sname=sname):
            for (s, v) in waits:
                e.wait_ge(sems[s], v)
            ins = fn(e)
            ins.then_inc(sems[sname], 1)

        self.prog[eng].append(run)
        self._commit((sname, val), reads, writes)

    def dma(self, eng, out, in_, slot, reads=(), writes=(), acc=False, track=(), **kw):
        reads = self._bl(reads)
        writes = self._bl(writes)
        track = self._bl(track)
        slot = slot.b if isinstance(slot, TB) else slot
        if slot.dsem is None:
            self.nd += 1
            slot.dsem = self._mksem("D%d" % self.nd)
        sname = slot.dsem
        waits = self._deps(eng, reads, writes)
        self.cnt[sname] += 16
        val = self.cnt[sname]
        sems = self.sems

        def run(e, waits=waits, sname=sname):
            for (s, v) in waits:
                e.wait_ge(sems[s], v)
            e.dma_start(out=out, in_=in_, **kw).then_inc(sems[sname], 16)

        self.prog[eng].append(run)
        self._commit((sname, val), reads, writes, acc=acc)
        for b in track:
            b.w.append((sname, val))

    def custom(self, eng, fn, slot, n_dma, reads=(), writes=(), acc=False, track=()):
        reads = self._bl(reads)
        writes = self._bl(writes)
        track = self._bl(track)
        slot = slot.b if isinstance(slot, TB) else slot
        if slot.dsem is None:
            self.nd += 1
            slot.dsem = self._mksem("D%d" % self.nd)
        sname = slot.dsem
        waits = self._deps(eng, reads, writes)
        self.cnt[sname] += 16 * n_dma
        val = self.cnt[sname]
        sems = self.sems

        def run(e, waits=waits, sname=sname):
            for (s, v) in waits:
                e.wait_ge(sems[s], v)
            fn(e, sems[sname])

        self.prog[eng].append(run)
        self._commit((sname, val), reads, writes, acc=acc)
        for b in track:
            b.w.append((sname, val))

    def barrier(self):
        snap = dict(self.cnt)
        sems = self.sems
        for eng in self.engs:
            waits = []
            for s, v in snap.items():
                if v == 0:
                    continue
                if eng == "pe" and s == "E_pe":
                    continue
                if self.waited[eng].get(s, 0) >= v:
                    continue
                self.waited[eng][s] = v
                waits.append((s, v))

            def run(e, waits=waits):
                for (s, v) in waits:
                    e.wait_ge(sems[s], v)
            self.prog[eng].append(run)

    def emit(self):
        nc = self.nc
        prog = self.prog
        with nc.Block() as block:
            @block.tensor
            def _(e):
                for f in prog["pe"]:
                    f(e)

            @block.scalar
            def _(e):
                for f in prog["act"]:
                    f(e)

            @block.vector
            def _(e):
                for f in prog["dve"]:
                    f(e)

            @block.gpsimd
            def _(e):
                for f in prog["pool"]:
                    f(e)

            @block.sync
            def _(e):
                for f in prog["sp"]:
                    f(e)
        self.prog = {k: [] for k in self.engs}


def host_consts():
    c = {}
    c["ident_bf"] = np.eye(128, dtype=np.float32).astype(ml_dtypes.bfloat16)
    c["ident_f"] = np.eye(128, dtype=np.float32)
    sw = np.zeros((128, 128), np.float32)
    for dp in range(128):
        sw[(dp + 64) % 128, dp] = 1.0
    c["swap_bf"] = sw.astype(ml_dtypes.bfloat16)
    d = np.arange(128)
    inv = 10000.0 ** (-(2.0 * (d % 64)) / 128.0)
    sgn = np.where(d < 64, -1.0, 1.0)
    vec = np.zeros((128, 4), np.float32)
    vec[:, 0] = inv.astype(np.float32)
    vec[:, 1] = sgn
    vec[:, 2] = d
    vec[:, 3] = np.where(d > 0, 1.0e5, 0.0)
    c["vec"] = vec
    c["ltri"] = (d[:, None] < d[None, :]).astype(np.float32)
    c["ones_f"] = np.ones((128, 128), np.float32)
    c["kcoff"] = (np.arange(8)[None, :] * 128 + d[:, None]).astype(np.float32)
    c["iota_b"] = np.tile(np.arange(128, dtype=np.float32)[None, :], (128, 1))
    H = 4
    gam = 1.0 - 2.0 ** (-5.0 - np.arange(H))
    lg = np.log(gam)
    idx = np.arange(128)
    scale = 128.0 ** -0.5
    dm = np.zeros((128, H, 128), np.float32)
    for h in range(H):
        rel = idx[None, :] - idx[:, None]
        dm[:, h, :] = np.where(rel >= 0, np.exp(lg[h] * np.maximum(rel, 0)), 0.0) * scale
    c["dmaskT"] = dm
    zeta = np.zeros((128, H), np.float32)
    for h in range(H):
        zeta[:, h] = np.exp(lg[h] * (127 - idx)) * scale
    c["zeta"] = zeta
    xi = np.zeros((128, H, 512), np.float32)
    for h in range(H):
        xi[:, h, :] = np.tile(np.exp(lg[h] * (idx + 1)), 4)[None, :]
    c["xi"] = xi.astype(ml_dtypes.bfloat16)
    c["_cdecay"] = [float(np.exp(lg[h] * 128)) for h in range(H)]
    tri = (idx[:, None] <= idx[None, :]).astype(np.float32)
    c["tri_bf"] = tri.astype(ml_dtypes.bfloat16)
    return c


CONSTS = host_consts()


def build(debug=False, stop=99):
    nc = bass.Bass("TRN2", target_bir_lowering=False)
    cdecay = CONSTS["_cdecay"]

    def din(name, shape, dt):
        return nc.dram_tensor(name, list(shape), dt, kind="ExternalInput").ap()

    x_d = din("x", [S_LEN, D], F32)
    c_d = din("c", [128, 8], F32)
    pos_d = din("positions", [1, S_LEN], I32)
    w_ada = din("w_ada", [D, 6 * D], F32)
    aux_d = din("aux", [1, 6 * D + 3 * D + 256 + 36], F32)
    w_in = din("w_in", [D, 8192], F32)
    w_ret_o = din("w_ret_o", [D, D], F32)
    w_diff_o = din("w_diff_o", [D, D], F32)
    w_out = din("w_out", [D, D], F32)
    lam_d = din("lam", [4, 128], F32)
    w_rt = din("w_rt", [D, 36], F32)
    w_eg_rows = din("w_exp_gate", [N_EXP * 128, 8 * D_EXP], F32)
    w_eu_rows = din("w_exp_up", [N_EXP * 128, 8 * D_EXP], F32)
    w_ed_rows = din("w_exp_down", [N_EXP * 128, 4 * D], F32)
    k_ident_bf = din("ident_bf", [128, 128], BF16)
    k_ident_f = din("ident_f", [128, 128], F32)
    k_swap = din("swap_bf", [128, 128], BF16)
    k_vec = din("vec", [128, 4], F32)
    k_dmask = din("dmaskT", [128, 4, 128], F32)
    k_zeta = din("zeta", [128, 4], F32)
    k_xi = din("xi", [128, 4, 512], BF16)
    k_tri = din("tri_bf", [128, 128], BF16)
    k_ltri = din("ltri", [128, 128], F32)
    k_ones = din("ones_f", [128, 128], F32)
    k_kcoff = din("kcoff", [128, 8], F32)
    k_iota = din("iota_b", [128, 128], F32)

    out_d = nc.dram_tensor("out", [S_LEN, D], F32, kind="ExternalOutput").ap()
    skind = "ExternalOutput" if debug else "Internal"

    def dscr(name, shape, dt):
        return nc.dram_tensor(name, list(shape), dt, kind=skind).ap()

    hT_d = dscr("hT_d", [128, 8, S_LEN], BF16)
    roT_d = dscr("roT_d", [128, 8, S_LEN], BF16)
    doT_d = dscr("doT_d", [128, 8, S_LEN], BF16)
    mT_d = dscr("mT_d", [128, 8, S_LEN], BF16)
    NSUB = 2
    RB = 128 * NSUB
    NBIG = (2 * S_LEN) // RB + N_EXP
    NBLK = NBIG * NSUB
    xpad_d = dscr("xpad_d", [NBLK * 128, D], BF16)
    ypad_d = dscr("ypad_d", [NBLK * 128, D], F32)
    B_xpad, B_ypad = Buf("xpad"), Buf("ypad")
    B_xz = Buf("xpad_zero")
    wb_eg = dscr("wb_eg", [N_EXP * 128, 8 * D_EXP], BF16)
    wb_eu = dscr("wb_eu", [N_EXP * 128, 8 * D_EXP], BF16)
    wb_ed = dscr("wb_ed", [N_EXP * 128, 4 * D], BF16)
    B_wb = Buf("wb")
    cvt_list = [(dst, src, r) for (dst, src) in [(wb_eg, w_eg_rows), (wb_eu, w_eu_rows), (wb_ed, w_ed_rows)] for r in range(N_EXP)]
    cvt_pos = [0]
    x1_d = dscr("x1_d", [S_LEN, D], F32)
    B_hT, B_roT, B_doT, B_mT, B_h2T, B_x1, B_out, B_wd = (Buf(n) for n in
                                                         ["hT", "roT", "doT", "mT", "h2T", "x1", "out", "wd"])

    with ExitStack() as es:
        S = Sched(nc, es)
        uid = [0]

        def cvt_issue(n):
            for _ in range(n):
                if cvt_pos[0] >= len(cvt_list):
                    return
                dst, src, r = cvt_list[cvt_pos[0]]
                cvt_pos[0] += 1
                S.dma("pool", dst[r * 128:(r + 1) * 128, :], src[r * 128:(r + 1) * 128, :], B_wb, writes=[B_wb], acc=True)

        def sb(stack, shape, dt, name=None):
            uid[0] += 1
            nm = (name or "t") + "_%d" % uid[0]
            return TB(stack.enter_context(nc.sbuf_tensor(nm, list(shape), dt)), nm)

        def ps(stack, shape, dt, name=None):
            uid[0] += 1
            nm = (name or "p") + "_%d" % uid[0]
            return TB(stack.enter_context(nc.psum_tensor(nm, list(shape), dt)), nm)

        ident = sb(es, [128, 128], BF16, "ident")
        identf = sb(es, [128, 128], F32, "identf")
        vec = sb(es, [128, 4], F32, "vec")
        modT = sb(es, [128, 64], F32, "modT")
        A1 = sb(es, [128, 8], F32, "A1")
        A2 = sb(es, [128, 8], F32, "A2")
        g1b = sb(es, [128, D], F32, "g1b")
        g2b = sb(es, [128, D], F32, "g2b")
        nfb = sb(es, [128, D], F32, "nfb")
        dnb = sb(es, [128, 256], F32, "dnb")
        rbb = sb(es, [128, 36], F32, "rbb")
        neglam = sb(es, [128, 1], F32, "neglam")
        MM = sb(es, [128, NT, 2, 32], F32, "MM")
        wAB = sb(es, [128, NT, 2], F32, "wAB")
        A2b = sb(es, [128, D], F32, "A2b")
        B2b = sb(es, [128, D], F32, "B2b")
        ltri = sb(es, [128, 128], F32, "ltri")
        onesf = sb(es, [128, 128], F32, "onesf")
        kcoff = sb(es, [128, 8], F32, "kcoff")
        iota_b = sb(es, [128, 128], F32, "iota_b")
        desti = sb(es, [128, NT, 2], I32, "desti")
        widx1 = sb(es, [128, NBIG], I32, "widx1")
        wrt = sb(es, [128, 8, 36], F32, "wrt")
        ones_row = sb(es, [1, 128], F32, "ones_row")
        es_rope = ExitStack()
        swapm = sb(es_rope, [128, 128], BF16, "swap")
        dmask = sb(es_rope, [128, 4, 128], F32, "dmask")
        zeta = sb(es_rope, [128, 4], F32, "zeta")
        xi = sb(es_rope, [128, 4, 512], BF16, "xi")
        tri = sb(es_rope, [128, 128], BF16, "tri")
        cosT = sb(es_rope, [128, S_LEN], BF16, "cosT")
        sinT = sb(es_rope, [128, S_LEN], BF16, "sinT")

        for (t, src) in [(ident, k_ident_bf), (identf, k_ident_f), (swapm, k_swap), (vec, k_vec), (dmask, k_dmask),
                         (zeta, k_zeta), (xi, k_xi), (tri, k_tri), (ltri, k_ltri), (onesf, k_ones), (kcoff, k_kcoff), (iota_b, k_iota)]:
            S.dma("sp", t[:], src, t, writes=[t])
        S.dma("sp", wrt[:], w_rt.rearrange("(kc p) n -> p kc n", p=128), wrt, writes=[wrt])
        S.op("dve", lambda e: e.memset(ones_row[:], 1.0), writes=[ones_row])

        with ExitStack() as ph:
            scol = sb(ph, [128, 8], F32, "scol")
            ccol = sb(ph, [128, 8], F32, "ccol")
            modrow = sb(ph, [1, 6 * D + 3 * D + 256 + 36], F32, "modrow")
            auxrow = sb(ph, [1, 6 * D + 3 * D + 256 + 36], F32, "auxrow")
            wst = [sb(ph, [128, 8, 512], BF16, "wada%d" % i) for i in range(3)]
            scol_bf = sb(ph, [128, 8], BF16, "scol_bf")
            pm = [ps(ph, [1, 512], F32, "pmod%d" % i) for i in range(2)]
            pT = ps(ph, [128, 64], F32, "pT")
            pb = [ps(ph, [128, 512], F32, "pb%d" % i) for i in range(2)]

            S.dma("sp", ccol[:], c_d, ccol, writes=[ccol])
            S.dma("sp", auxrow[:], aux_d, auxrow, writes=[auxrow])
            S.op("act", lambda e: e.activation(out=scol[:], in_=ccol[:], func=AF.Silu), reads=[ccol], writes=[scol])
            S.op("dve", lambda e: e.tensor_copy(scol_bf[:], scol[:]), reads=[scol], writes=[scol_bf])
            HC = 2048
            posi = sb(ph, [128, HC], I32, "posi")
            ang = sb(ph, [128, HC], F32, "ang")
            kk = sb(ph, [128, HC], F32, "kk")
            a2 = sb(ph, [128, HC], F32, "a2")
            for hc in range(S_LEN // HC):
                sl = slice(hc * HC, (hc + 1) * HC)
                S.dma("sp", posi[:], pos_d[0:1, sl].partition_broadcast(128), posi, writes=[posi])
                S.op("dve", lambda e: e.tensor_copy(ang[:], posi[:]), reads=[posi], writes=[ang])
                S.op("dve", lambda e: e.tensor_scalar(out=ang[:], in0=ang[:], scalar1=vec[:, 0:1], scalar2=None,
                                                      op0=ALU.mult), reads=[ang, vec], writes=[ang])
                for which in range(2):
                    dst = sinT if which == 0 else cosT
                    if which == 1:
                        S.op("dve", lambda e: e.tensor_scalar(out=ang[:], in0=ang[:], scalar1=float(np.pi / 2),
                                                              scalar2=None, op0=ALU.add), reads=[ang], writes=[ang])
                    S.op("dve", lambda e: e.tensor_scalar(out=kk[:], in0=ang[:], scalar1=float(1 / (2 * np.pi)),
                                                          scalar2=MAGIC, op0=ALU.mult, op1=ALU.add),
                         reads=[ang], writes=[kk])
                    S.op("dve", lambda e: e.tensor_scalar(out=kk[:], in0=kk[:], scalar1=-MAGIC,
                                                          scalar2=float(-2 * np.pi), op0=ALU.add, op1=ALU.mult),
                         reads=[kk], writes=[kk])
                    S.op("dve", lambda e: e.tensor_tensor(out=a2[:], in0=ang[:], in1=kk[:], op=ALU.add),
                         reads=[ang, kk], writes=[a2])
                    S.op("dve", lambda e: e.tensor_scalar(out=a2[:], in0=a2[:], scalar1=float(np.pi),
                                                          scalar2=float(-np.pi), op0=ALU.min, op1=ALU.max),
                         reads=[a2], writes=[a2])
                    if which == 0:
                        S.op("act", lambda e: e.activation(out=a2[:], in_=a2[:], func=AF.Sin), reads=[a2], writes=[a2])
                        S.op("dve", lambda e, sl=sl: e.tensor_scalar(out=sinT[:, sl], in0=a2[:], scalar1=vec[:, 1:2],
                                                                     scalar2=None, op0=ALU.mult),
                             reads=[a2, vec], writes=[sinT])
                    else:
                        S.op("act", lambda e, sl=sl: e.activation(out=cosT[:, sl], in_=a2[:], func=AF.Sin),
                             reads=[a2], writes=[cosT])
            w_ada_v = w_ada.rearrange("(kc p) n -> p kc n", p=128)
            for j in range(12):
                w = wst[j % 3]
                S.dma("pool", w[:], w_ada_v[:, :, j * 512:(j + 1) * 512], w, writes=[w])
                p = pm[j % 2]

                def mm(e, w=w, p=p):
                    for k in range(8):
                        ins = e.matmul(p[:], lhsT=scol_bf[:, k:k + 1], rhs=w[:, k, :], start=(k == 0), stop=(k == 7))
                    return ins
                S.op("pe", mm, reads=[scol_bf, w], writes=[p])
                S.op("dve", lambda e, p=p, j=j: e.tensor_tensor(out=modrow[:, j * 512:(j + 1) * 512], in0=p[:],
                                                                in1=auxrow[:, j * 512:(j + 1) * 512], op=ALU.add),
                     reads=[p, auxrow], writes=[modrow])
            S.op("dve", lambda e: e.tensor_copy(modrow[:, 6 * D:], auxrow[:, 6 * D:]), reads=[auxrow], writes=[modrow])

            def mmT(e):
                for j in range(64):
                    ins = e.matmul(pT[:, j:j + 1], lhsT=modrow[:, j * 128:(j + 1) * 128], rhs=ones_row[:, 0:1],
                                   start=True, stop=True)
                return ins
            S.op("pe", mmT, reads=[modrow, ones_row], writes=[pT])
            S.op("dve", lambda e: e.tensor_copy(modT[:], pT[:]), reads=[pT], writes=[modT])
            S.op("dve", lambda e: e.scalar_tensor_tensor(out=A1[:], in0=modT[:, 8:16], scalar=1.0, in1=modT[:, 48:56],
                                                         op0=ALU.add, op1=ALU.mult), reads=[modT], writes=[A1])
            S.op("dve", lambda e: e.scalar_tensor_tensor(out=A2[:], in0=modT[:, 32:40], scalar=1.0, in1=modT[:, 56:64],
                                                         op0=ALU.add, op1=ALU.mult), reads=[modT], writes=[A2])

            def bcast(dst, off, n, i, mul=None):
                p = pb[i % 2]

                def mm(e, p=p):
                    return e.matmul(p[:, 0:n], lhsT=ones_row[:, :], rhs=modrow[:, off:off + n], start=True, stop=True)
                S.op("pe", mm, reads=[modrow, ones_row], writes=[p])
                if mul is None:
                    S.op("dve", lambda e, p=p: e.tensor_copy(dst, p[:, 0:n]), reads=[p], writes=[])
                else:
                    S.op("dve", lambda e, p=p: e.tensor_scalar(out=dst, in0=p[:, 0:n], scalar1=mul, scalar2=None,
                                                               op0=ALU.mult), reads=[p], writes=[])
            i = 0
            for half in range(2):
                bcast(g1b[:, half * 512:(half + 1) * 512], 2 * D + half * 512, 512, i); i += 1
                bcast(g2b[:, half * 512:(half + 1) * 512], 5 * D + half * 512, 512, i); i += 1
                bcast(nfb[:, half * 512:(half + 1) * 512], 8 * D + half * 512, 512, i); i += 1
            bcast(dnb[:], 9 * D, 256, i, mul=(1.0 - LAMBDA_INIT)); i += 1
            S.op("dve", lambda e: e.scalar_tensor_tensor(out=modrow[:, 4 * D:5 * D], in0=modrow[:, 4 * D:5 * D], scalar=1.0,
                                                         in1=modrow[:, 7 * D:8 * D], op0=ALU.add, op1=ALU.mult),
                 reads=[modrow, pT], writes=[modrow])
            for half in range(2):
                bcast(A2b[:, half * 512:(half + 1) * 512], 4 * D + half * 512, 512, i); i += 1
                bcast(B2b[:, half * 512:(half + 1) * 512], 3 * D + half * 512, 512, i); i += 1
            bcast(rbb[:], 9 * D + 256, 36, i); i += 1

            lamt = sb(ph, [128, 4, 128], F32, "lamt")
            lprod = sb(ph, [128, 2, 128], F32, "lprod")
            lsum = sb(ph, [128, 2], F32, "lsum")
            for r in range(4):
                S.dma("sp", lamt[:, r, :], lam_d[r:r + 1, :].partition_broadcast(128), lamt, writes=[lamt], acc=(r > 0))
            S.op("dve", lambda e: e.tensor_tensor(out=lprod[:, 0, :], in0=lamt[:, 0, :], in1=lamt[:, 1, :], op=ALU.mult),
                 reads=[lamt], writes=[lprod])
            S.op("dve", lambda e: e.tensor_tensor(out=lprod[:, 1, :], in0=lamt[:, 2, :], in1=lamt[:, 3, :], op=ALU.mult),
                 reads=[lamt, lprod], writes=[lprod])
            S.op("dve", lambda e: e.reduce_sum(out=lsum[:], in_=lprod[:], axis=AX.X), reads=[lprod], writes=[lsum])
            S.op("act", lambda e: e.activation(out=lsum[:], in_=lsum[:], func=AF.Exp), reads=[lsum], writes=[lsum])
            S.op("dve", lambda e: e.tensor_tensor(out=neglam[:], in0=lsum[:, 1:2], in1=lsum[:, 0:1], op=ALU.subtract),
                 reads=[lsum], writes=[neglam])
            S.op("dve", lambda e: e.tensor_scalar(out=neglam[:], in0=neglam[:], scalar1=-LAMBDA_INIT, scalar2=None,
                                                  op0=ALU.add), reads=[neglam], writes=[neglam])

            S.barrier()
            S.emit()

        def rstd_ops(ss, n, lnexp=False):
            S.op("dve", lambda e: e.tensor_scalar(out=ss[:], in0=ss[:], scalar1=1.0 / n, scalar2=EPS,
                                                  op0=ALU.mult, op1=ALU.add), reads=[ss], writes=[ss])
            if lnexp:
                S.op("act", lambda e: e.activation(out=ss[:], in_=ss[:], func=AF.Ln), reads=[ss], writes=[ss])
                S.op("act", lambda e: e.activation(out=ss[:], in_=ss[:], func=AF.Exp, scale=-0.5), reads=[ss], writes=[ss])
            else:
                S.op("act", lambda e: e.activation(out=ss[:], in_=ss[:], func=AF.Sqrt), reads=[ss], writes=[ss])
                S.op("dve", lambda e: e.reciprocal(out=ss[:], in_=ss[:]), reads=[ss], writes=[ss])

        es_w = ExitStack()
        Wh = [sb(es_w, [128, 8, 768], BF16, "Wh%d" % i) for i in range(2)]
        hTg = [sb(es_w, [128, 8, 512], BF16, "hTg%d" % i) for i in range(2)]
        w_in_v = w_in.rearrange("(kc p) n -> p kc n", p=128)

        def load_cols(W, cols):
            off = 0
            for ci, (c0, cn) in enumerate(cols):
                S.dma("pool", W[:, :, off:off + cn], w_in_v[:, :, c0:c0 + cn], W, writes=[W], acc=(ci > 0))
                off += cn

        def cols_ret(h):
            return [(h * 128, 128), (512 + h * 128, 128), (1024 + h * 256, 256), (2048 + h * 256, 256)]

        def cols_diff(h):
            return [(3072 + (2 * h) * 128, 256), (4096 + (2 * h) * 128, 256), (5120 + h * 256, 256)]
        load_cols(Wh[0], cols_ret(0))

        with ExitStack() as ph:
            xt = [sb(ph, [128, D], F32, "xt%d" % i) for i in range(3)]
            junk = sb(ph, [128, D], BF16, "junk")
            xn = [sb(ph, [128, D], BF16, "xn%d" % i) for i in range(3)]
            ssq = [sb(ph, [128, 1], F32, "ss%d" % i) for i in range(3)]
            pT = [ps(ph, [128, 8, 128], BF16, "pT%d" % i) for i in range(3)]
            hg = [sb(ph, [128, 8, 512], BF16, "hg%d" % i) for i in range(2)]
            def p1_a(t):
                x_ = xt[t % 3]
                ss = ssq[t % 3]
                xn_ = xn[t % 3]
                S.dma("sp", x_[:], x_d[t * 128:(t + 1) * 128, :], x_, writes=[x_])
                S.op("act", lambda e: e.activation(out=junk[:], in_=x_[:], func=AF.Square, accum_out=ss[:]),
                     reads=[x_], writes=[junk, ss])
                rstd_ops(ss, D)
                S.op("dve", lambda e: e.tensor_scalar(out=xn_[:], in0=x_[:], scalar1=ss[:, 0:1], scalar2=None, op0=ALU.mult),
                     reads=[x_, ss], writes=[xn_])

            def p1_b(t):
                xn_ = xn[t % 3]
                p_ = pT[t % 3]
                g = t // 4
                hg_ = hg[g % 2]

                def tr(e):
                    for c in range(8):
                        ins = e.transpose(p_[:, c, :], xn_[:, c * 128:(c + 1) * 128], ident[:])
                    return ins
                S.op("pe", tr, reads=[xn_, ident], writes=[p_])
                for c in range(8):
                    if c % 2 == 0:
                        S.op("act", lambda e, c=c: e.activation(
                            out=hg_[:, c, (t % 4) * 128:(t % 4 + 1) * 128], in_=p_[:, c, :], func=AF.Identity,
                            bias=modT[:, c:c + 1], scale=A1[:, c:c + 1]), reads=[p_], writes=[hg_])
                    else:
                        S.op("dve", lambda e, c=c: e.tensor_scalar(
                            out=hg_[:, c, (t % 4) * 128:(t % 4 + 1) * 128], in0=p_[:, c, :], scalar1=A1[:, c:c + 1],
                            scalar2=modT[:, c:c + 1], op0=ALU.mult, op1=ALU.add), reads=[p_], writes=[hg_])
                if t % 4 == 3:
                    S.dma("pool", hT_d[:, :, g * 512:(g + 1) * 512], hg_[:], hg_, reads=[hg_], writes=[B_hT], acc=True)

            p1_a(0)
            p1_a(1)
            for t in range(NT):
                p1_b(t)
                if t + 2 < NT:
                    p1_a(t + 2)
            S.barrier()
            S.emit()
        if stop <= 1:
            S.emit()
            es_w.close()
            es_rope.close()
            return nc

        rope_ctr = [0]

        def rope_evac(ph_tiles, p_in, g, outs, reads_extra=()):
            rope_ctr[0] += 1
            qsb, pswp, t1, t2 = ph_tiles[rope_ctr[0] % len(ph_tiles)]
            sl = slice(g * 512, (g + 1) * 512)
            S.op("act", lambda e: e.activation(out=qsb[:], in_=p_in[:], func=AF.Copy), reads=[p_in], writes=[qsb])
            S.op("pe", lambda e: e.matmul(pswp[:], lhsT=swapm[:], rhs=qsb[:], start=True, stop=True),
                 reads=[qsb, swapm], writes=[pswp])
            S.op("dve", lambda e: e.tensor_tensor(out=t1[:], in0=qsb[:], in1=cosT[:, sl], op=ALU.mult),
                 reads=[qsb], writes=[t1])
            S.op("dve", lambda e: e.tensor_tensor(out=t2[:], in0=pswp[:], in1=sinT[:, sl], op=ALU.mult),
                 reads=[pswp], writes=[t2])
            for (dst, dbuf, mul) in outs:
                if mul is None:
                    S.op("dve", lambda e, dst=dst: e.tensor_tensor(out=dst, in0=t1[:], in1=t2[:], op=ALU.add),
                         reads=[t1, t2], writes=[dbuf])
                else:
                    S.op("dve", lambda e: e.tensor_tensor(out=t1[:], in0=t1[:], in1=t2[:], op=ALU.add),
                         reads=[t1, t2], writes=[t1])
                    S.op("dve", lambda e, dst=dst, mul=mul: e.tensor_tensor(out=dst, in0=t1[:], in1=mul, op=ALU.mult),
                         reads=[t1], writes=[dbuf])

        hT_v = hT_d

        with ExitStack() as ph:
            rqT = sb(ph, [128, S_LEN], BF16, "rqT")
            rqxT = sb(ph, [128, S_LEN], BF16, "rqxT")
            rkT = sb(ph, [128, S_LEN], BF16, "rkT")
            rv = sb(ph, [128, NT, 256], BF16, "rv")
            srgT = sb(ph, [128, 2, S_LEN], BF16, "srgT")
            roT = sb(ph, [128, 2, S_LEN], BF16, "roT")
            qsb = [sb(ph, [128, 512], BF16, "qsb%d" % i) for i in range(2)]
            t1 = [sb(ph, [128, 512], F32, "t1%d" % i) for i in range(2)]
            t2 = [sb(ph, [128, 512], F32, "t2%d" % i) for i in range(2)]
            pA = [ps(ph, [128, 512], F32, "pA%d" % i) for i in range(2)]
            pswp = ps(ph, [128, 512], F32, "pswp")
            pS = ps(ph, [128, 128], F32, "pS")
            pKT = ps(ph, [128, 128], BF16, "pKT")
            pO = ps(ph, [128, 256], F32, "pO")
            pKV = ps(ph, [128, 256], F32, "pKV")
            pRT = ps(ph, [128, 2, 128], BF16, "pRT")
            PT2 = [sb(ph, [128, 128], BF16, "PT%d" % i) for i in range(2)]
            kz2 = [sb(ph, [128, 128], BF16, "kz%d" % i) for i in range(2)]
            state = sb(ph, [128, 256], F32, "state")
            stbf = [sb(ph, [128, 256], BF16, "stbf%d" % i) for i in range(3)]
            on2 = [sb(ph, [128, 256], BF16, "on%d" % i) for i in range(2)]
            ojunk = sb(ph, [128, 256], BF16, "ojunk")
            oss2 = [sb(ph, [128, 1], F32, "oss%d" % i) for i in range(2)]
            rt = [(qsb[i], pswp, t1[i], t2[i]) for i in range(2)]
            pai = [0]

            def nextp():
                pai[0] += 1
                return pA[pai[0] % 2]

            for h in range(4):
                W = Wh[h % 2]
                if h + 1 < 4:
                    load_cols(Wh[(h + 1) % 2], cols_ret(h + 1))
                else:
                    load_cols(Wh[(h + 1) % 2], cols_diff(0))
                for g in range(NG):
                    hT_ = hTg[g % 2]
                    sl = slice(g * 512, (g + 1) * 512)
                    S.dma("sp", hT_[:], hT_v[:, :, sl], hT_, reads=[B_hT], writes=[hT_])
                    cvt_issue(0)

                    def proj_fm(p, c0, W=W, hT_=hT_):
                        def mm(e):
                            for k in range(8):
                                ins = e.matmul(p[:], lhsT=W[:, k, c0:c0 + 128], rhs=hT_[:, k, :], start=(k == 0), stop=(k == 7))
                            return ins
                        S.op("pe", mm, reads=[W, hT_], writes=[p])
                    steps = []
                    steps.append((lambda p: proj_fm(p, 0),
                                  lambda p, g=g, sl=sl, h=h: rope_evac(rt, p, g, [(rqT[:, sl], rqT, None), (rqxT[:, sl], rqxT, xi[:, h, :])])))
                    steps.append((lambda p: proj_fm(p, 128),
                                  lambda p, g=g, sl=sl: rope_evac(rt, p, g, [(rkT[:, sl], rkT, None)])))
                    for c in range(2):
                        steps.append((lambda p, c=c: proj_fm(p, 512 + c * 128),
                                      lambda p, c=c, sl=sl: S.op("act", lambda e: e.activation(out=srgT[:, c, sl], in_=p[:], func=AF.Silu),
                                                                 reads=[p], writes=[srgT])))
                    for tt in range(4):
                        def pj(p, tt=tt, W=W, hT_=hT_):
                            def mmv(e):
                                for k in range(8):
                                    ins = e.matmul(p[:, 0:256], lhsT=hT_[:, k, tt * 128:(tt + 1) * 128], rhs=W[:, k, 256:512],
                                                   start=(k == 0), stop=(k == 7))
                                return ins
                            S.op("pe", mmv, reads=[W, hT_], writes=[p])
                        steps.append((pj, lambda p, tt=tt, g=g: S.op("act", lambda e: e.activation(out=rv[:, g * 4 + tt, :], in_=p[:, 0:256], func=AF.Copy),
                                                                     reads=[p], writes=[rv])))
                    ps_ = [nextp() for _ in steps]
                    steps[0][0](ps_[0])
                    for i in range(len(steps)):
                        if i + 1 < len(steps):
                            steps[i + 1][0](ps_[i + 1])
                        steps[i][1](ps_[i])
                def st_x(n, h=h):
                    cs = slice(n * 128, (n + 1) * 128)
                    i2 = n % 2
                    S.op("pe", lambda e: e.matmul(pS[:], lhsT=rkT[:, cs], rhs=rqT[:, cs], start=True, stop=True),
                         reads=[rkT, rqT], writes=[pS])
                    S.op("dve", lambda e: e.tensor_tensor(out=PT2[i2][:], in0=pS[:], in1=dmask[:, h, :], op=ALU.mult),
                         reads=[pS, dmask], writes=[PT2[i2]])
                    if n < NT - 1:
                        S.op("pe", lambda e: e.transpose(pKT[:], rkT[:, cs], ident[:]), reads=[rkT, ident], writes=[pKT])
                        S.op("act", lambda e: e.activation(out=kz2[i2][:], in_=pKT[:], func=AF.Identity, scale=zeta[:, h:h + 1]),
                             reads=[pKT, zeta], writes=[kz2[i2]])

                def st_y(n, h=h):
                    cs = slice(n * 128, (n + 1) * 128)
                    i2, i3 = n % 2, n % 3
                    po_ = pA[i2]

                    def mmo(e):
                        ins = e.matmul(po_[:, 0:256], lhsT=PT2[i2][:], rhs=rv[:, n, :], start=True, stop=(n == 0))
                        if n > 0:
                            ins = e.matmul(po_[:, 0:256], lhsT=rqxT[:, cs], rhs=stbf[(n - 1) % 3][:], start=False, stop=True)
                        return ins
                    S.op("pe", mmo, reads=[PT2[i2], rv, rqxT] + ([stbf[(n - 1) % 3]] if n > 0 else []), writes=[po_])
                    if n < NT - 1:
                        S.op("pe", lambda e: e.matmul(pKV[:], lhsT=kz2[i2][:], rhs=rv[:, n, :], start=True, stop=True),
                             reads=[kz2[i2], rv], writes=[pKV])
                        if n == 0:
                            S.op("dve", lambda e: e.tensor_copy(stbf[i3][:], pKV[:]), reads=[pKV], writes=[stbf[i3]])
                            S.op("dve", lambda e: e.tensor_copy(state[:], pKV[:]), reads=[pKV], writes=[state])
                        else:
                            S.op("dve", lambda e: e.scalar_tensor_tensor(out=stbf[i3][:], in0=state[:], scalar=cdecay[h], in1=pKV[:],
                                                                         op0=ALU.mult, op1=ALU.add), reads=[state, pKV], writes=[stbf[i3]])
                            if n < NT - 2:
                                S.op("dve", lambda e: e.scalar_tensor_tensor(out=state[:], in0=state[:], scalar=cdecay[h], in1=pKV[:],
                                                                             op0=ALU.mult, op1=ALU.add), reads=[state, pKV], writes=[state])
                    oss_ = oss2[i2]
                    S.op("act", lambda e: e.activation(out=ojunk[:], in_=po_[:, 0:256], func=AF.Square, accum_out=oss_[:]),
                         reads=[po_], writes=[ojunk, oss_])
                    rstd_ops(oss_, 256, lnexp=True)
                    S.op("dve", lambda e: e.tensor_scalar(out=on2[i2][:], in0=po_[:, 0:256], scalar1=oss_[:, 0:1], scalar2=None, op0=ALU.mult),
                         reads=[po_, oss_], writes=[on2[i2]])

                def st_z(n):
                    cs = slice(n * 128, (n + 1) * 128)
                    i2 = n % 2

                    def trO(e):
                        for c in range(2):
                            ins = e.transpose(pRT[:, c, :], on2[i2][:, c * 128:(c + 1) * 128], ident[:])
                        return ins
                    S.op("pe", trO, reads=[on2[i2], ident], writes=[pRT])
                    S.op("dve", lambda e: e.tensor_tensor(out=roT[:, :, cs], in0=pRT[:], in1=srgT[:, :, cs], op=ALU.mult),
                         reads=[pRT, srgT], writes=[roT])

                st_x(0)
                st_y(0)
                for n in range(NT):
                    if n + 1 < NT:
                        st_x(n + 1)
                        st_y(n + 1)
                    st_z(n)
                S.dma("pool", roT_d[:, 2 * h:2 * h + 2, :], roT[:], roT, reads=[roT], writes=[B_roT], acc=True)
            S.barrier()
            S.emit()
        if stop <= 2:
            S.emit()
            es_w.close()
            es_rope.close()
            return nc

        with ExitStack() as ph:
            dqT = sb(ph, [128, 2, S_LEN], BF16, "dqT")
            dkT = sb(ph, [128, 2, S_LEN], BF16, "dkT")
            dv = sb(ph, [128, NT, 258], BF16, "dv")
            doT = sb(ph, [128, 2, S_LEN], BF16, "doT")
            qsb = [sb(ph, [128, 512], BF16, "qsb%d" % i) for i in range(2)]
            t1 = [sb(ph, [128, 512], F32, "t1%d" % i) for i in range(2)]
            t2 = [sb(ph, [128, 512], F32, "t2%d" % i) for i in range(2)]
            pA = [ps(ph, [128, 512], F32, "pA%d" % i) for i in range(2)]
            pswp = ps(ph, [128, 512], F32, "pswp")
            pOa = [[ps(ph, [128, 512], F32, "pO%d%d" % (qb, c)) for c in range(2)] for qb in range(2)]
            pDT = ps(ph, [128, 2, 128], BF16, "pDT")
            PTs = [sb(ph, [128, 2, 256], BF16, "PT%d" % i) for i in range(4)]
            sc_ctr = [0]
            rl = sb(ph, [128, 2], F32, "rl")
            a1 = sb(ph, [128, 256], F32, "a1")
            a2_ = sb(ph, [128, 256], F32, "a2")
            ajunk = sb(ph, [128, 256], BF16, "ajunk")
            ass_ = sb(ph, [128, 1], F32, "ass")
            don = sb(ph, [128, 256], BF16, "don")
            rt = [(qsb[i], pswp, t1[i], t2[i]) for i in range(2)]
            pai = [0]

            def nextp3():
                pai[0] += 1
                return pA[pai[0] % 2]

            osb = [sb(ph, [128, 2, 2, 257], F32, "osb%d" % i) for i in range(2)]
            pending = [None]

            dons = [[sb(ph, [128, 256], BF16, "don%d%d" % (i, j)) for j in range(2)] for i in range(2)]
            rl2 = [sb(ph, [128, 2], F32, "rl%d" % i) for i in range(2)]
            a1_2 = [sb(ph, [128, 256], F32, "a1_%d" % i) for i in range(2)]
            a2_2 = [sb(ph, [128, 256], F32, "a2_%d" % i) for i in range(2)]
            ass2 = [sb(ph, [128, 1], F32, "ass%d" % i) for i in range(2)]
            ajk2 = [sb(ph, [128, 256], BF16, "ajk%d" % i) for i in range(2)]
            micro = []

            def make_epilogue(osb_, q0, par):
                st = []
                for qb in range(2):
                    don_ = dons[par][qb]
                    rl_, a1_, a2q, ass_q, ajk = rl2[qb], a1_2[qb], a2_2[qb], ass2[qb], ajk2[qb]
                    tok = slice((q0 + qb) * 128, (q0 + qb + 1) * 128)

                    def m1(qb=qb, rl_=rl_, a1_=a1_, a2q=a2q):
                        S.op("dve", lambda e: e.reciprocal(out=rl_[:], in_=osb_[:, qb, :, 256]), reads=[osb_, rl_], writes=[rl_])
                        S.op("dve", lambda e: e.tensor_tensor(out=rl_[:, 1:2], in0=rl_[:, 1:2], in1=neglam[:], op=ALU.mult),
                             reads=[rl_, neglam], writes=[rl_])
                        S.op("dve", lambda e: e.tensor_scalar(out=a1_[:], in0=osb_[:, qb, 0, 0:256], scalar1=rl_[:, 0:1], scalar2=None,
                                                              op0=ALU.mult), reads=[osb_, rl_], writes=[a1_])
                        S.op("dve", lambda e: e.scalar_tensor_tensor(out=a2q[:], in0=osb_[:, qb, 1, 0:256], scalar=rl_[:, 1:2], in1=a1_[:],
                                                                     op0=ALU.mult, op1=ALU.add), reads=[osb_, rl_, a1_], writes=[a2q])

                    def m2(a2q=a2q, ass_q=ass_q, ajk=ajk):
                        S.op("act", lambda e: e.activation(out=ajk[:], in_=a2q[:], func=AF.Square, accum_out=ass_q[:]),
                             reads=[a2q], writes=[ajk, ass_q])

                    def m3(ass_q=ass_q):
                        S.op("dve", lambda e: e.tensor_scalar(out=ass_q[:], in0=ass_q[:], scalar1=1.0 / 256, scalar2=EPS,
                                                              op0=ALU.mult, op1=ALU.add), reads=[ass_q], writes=[ass_q])

                    def m4(ass_q=ass_q):
                        S.op("act", lambda e: e.activation(out=ass_q[:], in_=ass_q[:], func=AF.Ln), reads=[ass_q], writes=[ass_q])
                        S.op("act", lambda e: e.activation(out=ass_q[:], in_=ass_q[:], func=AF.Exp, scale=-0.5), reads=[ass_q], writes=[ass_q])

                    def m5(a2q=a2q, ass_q=ass_q, don_=don_):
                        S.op("dve", lambda e: e.scalar_tensor_tensor(out=don_[:], in0=a2q[:], scalar=ass_q[:, 0:1], in1=dnb[:],
                                                                     op0=ALU.mult, op1=ALU.mult), reads=[a2q, ass_q, dnb], writes=[don_])

                    def m6(don_=don_, tok=tok):
                        def trD(e):
                            for c in range(2):
                                ins = e.transpose(pDT[:, c, :], don_[:, c * 128:(c + 1) * 128], ident[:])
                            return ins
                        S.op("pe", trD, reads=[don_, ident], writes=[pDT])
                        S.op("dve", lambda e: e.tensor_copy(doT[:, :, tok], pDT[:]), reads=[pDT], writes=[doT])
                    st.append([m1, m2, m3, m4, m5, m6])
                out = []
                for k in range(6):
                    out.append(st[0][k])
                    out.append(st[1][k])
                return out

            S.op("pool", lambda e: e.memset(dv[:, :, 256:258], 1.0), writes=[dv])
            for h in range(4):
                W = Wh[h % 2]
                if h + 1 < 4:
                    load_cols(Wh[(h + 1) % 2], cols_diff(h + 1))
                for g in range(NG):
                    hT_ = hTg[g % 2]
                    sl = slice(g * 512, (g + 1) * 512)
                    S.dma("sp", hT_[:], hT_v[:, :, sl], hT_, reads=[B_hT], writes=[hT_])
                    cvt_issue(3)
                    steps = []
                    for (dst, c0) in [(dqT, 0), (dkT, 256)]:
                        for c in range(2):
                            def pj(p, cc=c0 + c * 128, W=W, hT_=hT_):
                                def mm(e):
                                    for k in range(8):
                                        ins = e.matmul(p[:], lhsT=W[:, k, cc:cc + 128], rhs=hT_[:, k, :], start=(k == 0), stop=(k == 7))
                                    return ins
                                S.op("pe", mm, reads=[W, hT_], writes=[p])
                            steps.append((pj, lambda p, dst=dst, c=c, g=g, sl=sl: rope_evac(rt, p, g, [(dst[:, c, sl], dst, None)])))
                    for tt in range(4):
                        def pjv(p, tt=tt, W=W, hT_=hT_):
                            def mmv(e):
                                for k in range(8):
                                    ins = e.matmul(p[:, 0:256], lhsT=hT_[:, k, tt * 128:(tt + 1) * 128], rhs=W[:, k, 512:768],
                                                   start=(k == 0), stop=(k == 7))
                                return ins
                            S.op("pe", mmv, reads=[W, hT_], writes=[p])
                        steps.append((pjv, lambda p, tt=tt, g=g: S.op("act", lambda e: e.activation(out=dv[:, g * 4 + tt, 0:256], in_=p[:, 0:256], func=AF.Copy),
                                                                      reads=[p], writes=[dv])))
                    ps_ = [nextp3() for _ in steps]
                    steps[0][0](ps_[0])
                    for i in range(len(steps)):
                        if i + 1 < len(steps):
                            steps[i + 1][0](ps_[i + 1])
                        steps[i][1](ps_[i])
                sc_scale = 128.0 ** -0.5
                for qg in range(S_LEN // 256):
                    q0 = 2 * qg
                    nkb = q0 + 2
                    items = list(range(nkb))

                    def score(jb, qg=qg, q0=q0):
                        sc_ctr[0] += 1
                        p = [pA[0], pA[1], pswp][sc_ctr[0] % 3]
                        pt = PTs[jb % 4]
                        last = (jb == q0 + 1)
                        qlo = 128 if last else 0
                        qs = slice(qg * 256 + qlo, (qg + 1) * 256)
                        ks = slice(jb * 128, (jb + 1) * 128)

                        def mm(e):
                            for c in range(2):
                                ins = e.matmul(p[:, c * 256 + qlo:(c + 1) * 256], lhsT=dkT[:, c, ks], rhs=dqT[:, c, qs],
                                               start=True, stop=True)
                            return ins
                        S.op("pe", mm, reads=[dkT, dqT], writes=[p])
                        if last:
                            for c in range(2):
                                S.op("act", lambda e, c=c: e.activation(out=pt[:, c, 128:256], in_=p[:, c * 256 + 128:(c + 1) * 256],
                                                                        func=AF.Exp, scale=sc_scale), reads=[p], writes=[pt])
                        else:
                            S.op("act", lambda e: e.activation(out=pt[:].rearrange("p c q -> p (c q)"), in_=p[:],
                                                               func=AF.Exp, scale=sc_scale), reads=[p], writes=[pt])
                        for qb in range(2):
                            if jb == q0 + qb:
                                for c in range(2):
                                    S.op("dve", lambda e, c=c, qb=qb: e.tensor_tensor(
                                        out=pt[:, c, qb * 128:(qb + 1) * 128], in0=pt[:, c, qb * 128:(qb + 1) * 128],
                                        in1=tri[:], op=ALU.mult), reads=[pt, tri], writes=[pt])

                    def pv(jb, q0=q0, nkb=nkb):
                        pt = PTs[jb % 4]
                        items_ = [(pOa[qb][c], c, qb, q0 + qb) for qb in range(2) if jb <= q0 + qb for c in range(2)]

                        def mmpv(e):
                            for (po, c, qb, lastk) in items_:
                                ins = e.matmul(po[:, 0:257], lhsT=pt[:, c, qb * 128:(qb + 1) * 128], rhs=dv[:, jb, 0:257],
                                               start=(jb == 0), stop=(jb == lastk))
                            return ins
                        S.op("pe", mmpv, reads=[pt, dv], writes=[it[0] for it in items_])
                    score(0)
                    if nkb > 1:
                        score(1)
                    for jb in items:
                        if jb + 2 < nkb:
                            score(jb + 2)
                        pv(jb)
                        if micro and jb >= 1:
                            micro.pop(0)()
                    while micro:
                        micro.pop(0)()
                    osb_ = osb[qg % 2]
                    for qb in range(2):
                        for c in range(2):
                            po = pOa[qb][c]
                            if c == 0:
                                S.op("dve", lambda e, po=po, qb=qb, c=c, osb_=osb_: e.tensor_copy(osb_[:, qb, c, :], po[:, 0:257]),
                                     reads=[po], writes=[osb_])
                            else:
                                S.op("dve", lambda e, po=po, qb=qb, c=c, osb_=osb_: e.tensor_copy(osb_[:, qb, c, :], po[:, 0:257]),
                                     reads=[po, osb_], writes=[osb_])
                    micro.extend(make_epilogue(osb_, q0, qg % 2))
                while micro:
                    micro.pop(0)()
                S.dma("pool", doT_d[:, 2 * h:2 * h + 2, :], doT[:], doT, reads=[doT], writes=[B_doT], acc=True)
            S.barrier()
            S.emit()
        if stop <= 3:
            S.emit()
            es_w.close()
            es_rope.close()
            return nc

        es_w.close()
        es_rope.close()
        cvt_issue(1000)
        with ExitStack() as ph:
            Wro = sb(ph, [128, 8, D], BF16, "Wro")
            Wdo = sb(ph, [128, 8, D], BF16, "Wdo")
            Wg = sb(ph, [128, 8, 2048], BF16, "Wg")
            S.dma("pool", Wro[:], w_ret_o.rearrange("(kc p) n -> p kc n", p=128), Wro, writes=[Wro])
            S.dma("pool", Wdo[:], w_diff_o.rearrange("(kc p) n -> p kc n", p=128), Wdo, writes=[Wdo])
            S.dma("pool", Wg[:], w_in_v[:, :, 6144:8192], Wg, writes=[Wg])
            zt = sb(ph, [128, 4, D], BF16, "zt")
            S.op("pool", lambda e: e.memset(zt[:], 0.0), writes=[zt])
            xpad_v = xpad_d.rearrange("(a p) f -> p a f", p=128)
            zf_pos = [0]

            def zero_fill(n):
                for _ in range(n):
                    a = zf_pos[0]
                    if a >= NBLK // 4:
                        return
                    zf_pos[0] += 1
                    S.dma("sp", xpad_v[:, a * 4:(a + 1) * 4, :], zt[:], zt, reads=[zt], track=[B_xz, B_xpad])
            rg_ = [sb(ph, [128, 8, 512], BF16, "rg%d" % i) for i in range(2)]
            dg_ = [sb(ph, [128, 8, 512], BF16, "dg%d" % i) for i in range(2)]
            hg_ = [sb(ph, [128, 8, 512], BF16, "hg%d" % i) for i in range(2)]
            mg_ = [sb(ph, [128, 8, 512], BF16, "mg%d" % i) for i in range(2)]
            pp = [ps(ph, [128, 512], F32, "pp%d" % i) for i in range(8)]
            sg = [sb(ph, [128, 512], BF16, "sg%d" % i) for i in range(4)]
            m1 = [sb(ph, [128, 512], F32, "m1%d" % i) for i in range(2)]
            m2 = [sb(ph, [128, 512], F32, "m2%d" % i) for i in range(2)]
            it = 0
            for g in range(NG):
                sl = slice(g * 512, (g + 1) * 512)
                r_, d_, h_, m_ = rg_[g % 2], dg_[g % 2], hg_[g % 2], mg_[g % 2]
                S.dma("sp", r_[:], roT_d[:, :, sl], r_, reads=[B_roT], writes=[r_])
                S.dma("sp", d_[:], doT_d[:, :, sl], d_, reads=[B_doT], writes=[d_])
                S.dma("sp", h_[:], hT_v[:, :, sl], h_, reads=[B_hT], writes=[h_])
                zero_fill((NBLK // 4 + NG - 1) // NG)
                for n in range(8):
                    P4 = pp[(it % 2) * 4:(it % 2) * 4 + 4]
                    sgr, sgd = sg[(it % 2) * 2], sg[(it % 2) * 2 + 1]
                    m1_, m2_ = m1[it % 2], m2[it % 2]
                    it += 1
                    ns = slice(n * 128, (n + 1) * 128)
                    for (p, Wt, src, c0) in [(P4[0], Wro, r_, 0), (P4[1], Wdo, d_, 0), (P4[2], Wg, h_, 0), (P4[3], Wg, h_, 1024)]:
                        def mm(e, p=p, Wt=Wt, src=src, c0=c0, n=n):
                            for k in range(8):
                                ins = e.matmul(p[:], lhsT=Wt[:, k, c0 + n * 128:c0 + (n + 1) * 128], rhs=src[:, k, :],
                                               start=(k == 0), stop=(k == 7))
                            return ins
                        S.op("pe", mm, reads=[Wt, src], writes=[p])
                    S.op("act", lambda e, p=P4[2], o=sgr: e.activation(out=o[:], in_=p[:], func=AF.Sigmoid), reads=[P4[2]], writes=[sgr])
                    S.op("act", lambda e, p=P4[3], o=sgd: e.activation(out=o[:], in_=p[:], func=AF.Sigmoid), reads=[P4[3]], writes=[sgd])
                    S.op("dve", lambda e, p=P4[0], o=m1_, s_=sgr: e.tensor_tensor(out=o[:], in0=p[:], in1=s_[:], op=ALU.mult),
                         reads=[P4[0], sgr], writes=[m1_])
                    S.op("dve", lambda e, p=P4[1], o=m2_, s_=sgd: e.tensor_tensor(out=o[:], in0=p[:], in1=s_[:], op=ALU.mult),
                         reads=[P4[1], sgd], writes=[m2_])
                    S.op("pool", lambda e, a=m1_, b=m2_, m_=m_, n=n: e.tensor_tensor(out=m_[:, n, :], in0=a[:], in1=b[:], op=ALU.add),
                         reads=[m1_, m2_], writes=[m_])
                S.dma("pool", mT_d[:, :, sl], m_[:], m_, reads=[m_], writes=[B_mT], acc=True)
            S.barrier()
            S.emit()
        if stop <= 4:
            S.emit()
            return nc

        with ExitStack() as ph:
            Wo = sb(ph, [128, 8, D], BF16, "Wo")
            S.dma("pool", Wo[:], w_out.rearrange("(kc p) n -> p kc n", p=128), Wo, writes=[Wo])
            for k in range(8):
                S.op("dve", lambda e, k=k: e.tensor_tensor(out=Wo[:, k, :], in0=Wo[:, k, :], in1=g1b[:], op=ALU.mult),
                     reads=[Wo], writes=[Wo])
            h2all = sb(ph, [128, NT, D], BF16, "h2all")
            h2b = [Buf("h2all%d" % i) for i in range(NT)]
            lgall = sb(ph, [128, NT, 36], F32, "lgall")
            lgb = [Buf("lg%d" % i) for i in range(NT)]
            ph2 = ExitStack()
            mg_ = [sb(ph2, [128, 8, 512], BF16, "mg%d" % i) for i in range(2)]
            xt = [sb(ph2, [128, D], F32, "xt%d" % i) for i in range(2)]
            x1t = [sb(ph2, [128, D], F32, "x1t%d" % i) for i in range(2)]
            xn2 = [sb(ph2, [128, D], F32, "xn2%d" % i) for i in range(2)]
            junk = sb(ph2, [128, D], BF16, "junk")
            ssq = [sb(ph2, [128, 1], F32, "ss%d" % i) for i in range(2)]
            h2f = [sb(ph2, [128, 8, 128], F32, "h2f%d" % i) for i in range(2)]
            po = [ps(ph2, [128, 512], F32, "po%d" % i) for i in range(4)]
            pT2 = [ps(ph2, [128, 4, 128], F32, "pT2%d" % i) for i in range(2)]
            pR = [ps(ph2, [128, 36], F32, "pR%d" % i) for i in range(2)]
            def p4_a1(t):
                g = t // 4
                tt = t % 4
                m_ = mg_[g % 2]
                if tt == 0:
                    S.dma("sp", m_[:], mT_d[:, :, g * 512:(g + 1) * 512], m_, reads=[B_mT], writes=[m_])
                x_ = xt[t % 2]
                x1_ = x1t[t % 2]
                S.dma("sp", x_[:], x_d[t * 128:(t + 1) * 128, :], x_, writes=[x_])
                for half in range(2):
                    p = po[(t % 2) * 2 + half]
                    hs = slice(half * 512, (half + 1) * 512)

                    def mm(e, p=p, hs=hs):
                        for k in range(8):
                            ins = e.matmul(p[:], lhsT=m_[:, k, tt * 128:(tt + 1) * 128], rhs=Wo[:, k, hs], start=(k == 0), stop=(k == 7))
                        return ins
                    S.op("pe", mm, reads=[m_, Wo], writes=[p])
                    S.op("dve", lambda e, p=p, hs=hs: e.tensor_tensor(out=x1_[:, hs], in0=p[:], in1=x_[:, hs], op=ALU.add),
                         reads=[p, x_], writes=[x1_])
                S.dma("sp", x1_d[t * 128:(t + 1) * 128, :], x1_[:], x1_, reads=[x1_], track=[B_x1])

            def p4_a2(t):
                x1_ = x1t[t % 2]
                xn_ = xn2[t % 2]
                ss = ssq[t % 2]
                S.op("act", lambda e: e.activation(out=junk[:], in_=x1_[:], func=AF.Square, accum_out=ss[:]),
                     reads=[x1_], writes=[junk, ss])
                rstd_ops(ss, D, lnexp=True)
                S.op("dve", lambda e: e.tensor_scalar(out=xn_[:], in0=x1_[:], scalar1=ss[:, 0:1], scalar2=None, op0=ALU.mult),
                     reads=[x1_, ss], writes=[xn_])

            def p4_b1(t):
                x1_ = x1t[t % 2]
                xn_ = xn2[t % 2]
                ss = ssq[t % 2]
                hf = h2f[t % 2]
                for hh in range(2):
                    p = pT2[hh]

                    def tr(e, p=p, hh=hh):
                        for c in range(4):
                            cc = hh * 4 + c
                            ins = e.transpose(p[:, c, :], xn_[:, cc * 128:(cc + 1) * 128], identf[:])
                        return ins
                    S.op("pe", tr, reads=[xn_, identf], writes=[p])
                    for c in range(4):
                        cc = hh * 4 + c
                        if c % 2 == 0:
                            S.op("act", lambda e, p=p, c=c, cc=cc: e.activation(
                                out=hf[:, cc, :], in_=p[:, c, :], func=AF.Identity, bias=modT[:, 24 + cc:25 + cc],
                                scale=A2[:, cc:cc + 1]), reads=[p], writes=[hf])
                        else:
                            S.op("dve", lambda e, p=p, c=c, cc=cc: e.tensor_scalar(
                                out=hf[:, cc, :], in0=p[:, c, :], scalar1=A2[:, cc:cc + 1], scalar2=modT[:, 24 + cc:25 + cc],
                                op0=ALU.mult, op1=ALU.add), reads=[p], writes=[hf])
                S.op("pool", lambda e: e.tensor_tensor(out=x1_[:], in0=xn_[:], in1=A2b[:], op=ALU.mult), reads=[xn_, x1_], writes=[x1_])
                S.op("pool", lambda e: e.tensor_tensor(out=h2all[:, t, :], in0=x1_[:], in1=B2b[:], op=ALU.add),
                     reads=[x1_], writes=[h2b[t]])

            def p4_b2(t):
                hf = h2f[t % 2]
                pr_ = pR[t % 2]

                def mmr(e):
                    for k in range(8):
                        ins = e.matmul(pr_[:], lhsT=hf[:, k, :], rhs=wrt[:, k, :], start=(k == 0), stop=(k == 7))
                    return ins
                S.op("pe", mmr, reads=[hf, wrt], writes=[pr_])
                S.op("dve", lambda e: e.tensor_tensor(out=lgall[:, t, :], in0=pr_[:], in1=rbb[:], op=ALU.add),
                     reads=[pr_], writes=[lgb[t]])

            p4_a1(0)
            p4_a2(0)
            for t in range(NT):
                if t + 1 < NT:
                    p4_a1(t + 1)
                p4_b1(t)
                if t + 1 < NT:
                    p4_a2(t + 1)
                if t >= 1:
                    p4_b2(t - 1)
            p4_b2(NT - 1)
            S.barrier()
            S.emit()
            ph2.close()
            V = "dve"
            gl = lgall[:, :, 0:4]
            el4 = lgall[:, :, 4:36].rearrange("p t (g j) -> p t g j", g=4)
            gmax = sb(ph, [128, NT], F32, "gmax")
            maskg = sb(ph, [128, NT, 4], F32, "maskg")
            gsh = sb(ph, [128, NT, 4], F32, "gsh")
            gsum = sb(ph, [128, NT], F32, "gsum")
            sel = sb(ph, [128, NT, 4, 8], F32, "sel")
            els = sb(ph, [128, NT, 8], F32, "els")
            el2 = sb(ph, [128, NT, 8], F32, "el2")
            mk = [sb(ph, [128, NT, 8], F32, "mk%d" % i) for i in range(2)]
            m12 = [sb(ph, [128, NT], F32, "m12_%d" % i) for i in range(2)]
            ee = sb(ph, [128, NT], F32, "ee")
            den = sb(ph, [128, NT], F32, "den")

            def bc(ap2, n):
                return ap2.unsqueeze(2).to_broadcast([128, NT, n])
            S.op(V, lambda e: e.reduce_max(out=gmax[:], in_=gl, axis=AX.X), reads=lgb, writes=[gmax])
            S.op(V, lambda e: e.tensor_tensor(out=maskg[:], in0=gl, in1=bc(gmax[:], 4), op=ALU.is_equal), reads=lgb + [gmax], writes=[maskg])
            S.op(V, lambda e: e.tensor_tensor(out=gsh[:], in0=gl, in1=bc(gmax[:], 4), op=ALU.subtract), reads=lgb + [gmax], writes=[gsh])
            S.op("act", lambda e: e.activation(out=gsh[:], in_=gsh[:], func=AF.Exp), reads=[gsh], writes=[gsh])
            S.op(V, lambda e: e.reduce_sum(out=gsum[:], in_=gsh[:], axis=AX.X), reads=[gsh], writes=[gsum])
            S.op(V, lambda e: e.reciprocal(out=gsum[:], in_=gsum[:]), reads=[gsum], writes=[gsum])
            S.op(V, lambda e: e.tensor_tensor(out=sel[:], in0=el4, in1=maskg[:].unsqueeze(3).to_broadcast([128, NT, 4, 8]), op=ALU.mult),
                 reads=lgb + [maskg], writes=[sel])
            S.op(V, lambda e: e.tensor_tensor(out=els[:], in0=sel[:, :, 0, :], in1=sel[:, :, 1, :], op=ALU.add), reads=[sel], writes=[els])
            S.op(V, lambda e: e.tensor_tensor(out=els[:], in0=els[:], in1=sel[:, :, 2, :], op=ALU.add), reads=[sel, els], writes=[els])
            S.op(V, lambda e: e.tensor_tensor(out=els[:], in0=els[:], in1=sel[:, :, 3, :], op=ALU.add), reads=[sel, els], writes=[els])
            S.op(V, lambda e: e.reduce_max(out=m12[0][:], in_=els[:], axis=AX.X), reads=[els], writes=[m12[0]])
            S.op(V, lambda e: e.tensor_tensor(out=mk[0][:], in0=els[:], in1=bc(m12[0][:], 8), op=ALU.is_equal), reads=[els, m12[0]], writes=[mk[0]])
            S.op(V, lambda e: e.scalar_tensor_tensor(out=el2[:], in0=mk[0][:], scalar=-1e30, in1=els[:], op0=ALU.mult, op1=ALU.add),
                 reads=[mk[0], els], writes=[el2])
            S.op(V, lambda e: e.reduce_max(out=m12[1][:], in_=el2[:], axis=AX.X), reads=[el2], writes=[m12[1]])
            S.op(V, lambda e: e.tensor_tensor(out=mk[1][:], in0=el2[:], in1=bc(m12[1][:], 8), op=ALU.is_equal), reads=[el2, m12[1]], writes=[mk[1]])
            S.op(V, lambda e: e.tensor_tensor(out=ee[:], in0=m12[1][:], in1=m12[0][:], op=ALU.subtract), reads=m12, writes=[ee])
            S.op("act", lambda e: e.activation(out=ee[:], in_=ee[:], func=AF.Exp), reads=[ee], writes=[ee])
            S.op(V, lambda e: e.tensor_scalar(out=den[:], in0=ee[:], scalar1=1.0, scalar2=None, op0=ALU.add), reads=[ee], writes=[den])
            S.op(V, lambda e: e.reciprocal(out=den[:], in_=den[:]), reads=[den], writes=[den])
            S.op(V, lambda e: e.tensor_tensor(out=wAB[:, :, 0], in0=den[:], in1=gsum[:], op=ALU.mult), reads=[den, gsum], writes=[wAB])
            S.op(V, lambda e: e.tensor_tensor(out=wAB[:, :, 1], in0=wAB[:, :, 0], in1=ee[:], op=ALU.mult), reads=[ee, wAB], writes=[wAB])
            for k in range(2):
                S.op(V, lambda e, k=k: e.tensor_tensor(
                    out=MM[:, :, k, :].rearrange("p t (g j) -> p t g j", g=4),
                    in0=maskg[:].unsqueeze(3).to_broadcast([128, NT, 4, 8]),
                    in1=mk[k][:].unsqueeze(2).to_broadcast([128, NT, 4, 8]), op=ALU.mult),
                    reads=[maskg, mk[k], MM], writes=[MM])
            rank = sb(ph, [128, NT, 32], F32, "rank")
            msall = sb(ph, [128, NT, 32], F32, "msall")
            csa = sb(ph, [128, NT, 32], F32, "csa")
            csb = sb(ph, [128, NT, 32], F32, "csb")
            pcs = sb(ph, [128, NT, 32], F32, "pcs")
            base = sb(ph, [128, 32], F32, "base")
            ppr = [ps(ph, [128, 512], F32, "ppr%d" % i) for i in range(2)]
            ppc = [ps(ph, [128, 512], F32, "ppc%d" % i) for i in range(2)]
            S.op("dve", lambda e: e.tensor_tensor(out=msall[:], in0=MM[:, :, 0, :], in1=MM[:, :, 1, :], op=ALU.add), reads=[MM], writes=[msall])
            msf = msall[:].rearrange("p t e -> p (t e)")
            for hh in range(2):
                S.op("pe", lambda e, hh=hh: e.matmul(ppr[hh][:], lhsT=ltri[:], rhs=msf[:, hh * 512:(hh + 1) * 512], start=True, stop=True),
                     reads=[msall, ltri], writes=[ppr[hh]])
                S.op("pe", lambda e, hh=hh: e.matmul(ppc[hh][:], lhsT=onesf[:], rhs=msf[:, hh * 512:(hh + 1) * 512], start=True, stop=True),
                     reads=[msall, onesf], writes=[ppc[hh]])
                S.op("dve", lambda e, hh=hh: e.tensor_copy(pcs[:, hh * 16:(hh + 1) * 16, :].rearrange("p t e -> p (t e)"), ppc[hh][:]),
                     reads=[ppc[hh]], writes=[pcs])
            src, dst = pcs, csa
            sh = 1
            first = True
            while sh < NT:
                S.op("dve", lambda e, src=src, dst=dst, sh=sh: e.tensor_copy(dst[:, 0:sh, :], src[:, 0:sh, :]), reads=[src, dst], writes=[dst])
                S.op("dve", lambda e, src=src, dst=dst, sh=sh: e.tensor_tensor(out=dst[:, sh:NT, :], in0=src[:, sh:NT, :], in1=src[:, 0:NT - sh, :], op=ALU.add),
                     reads=[src, dst], writes=[dst])
                src, dst = dst, (csb if dst is csa else csa)
                sh *= 2
            incl = src
            S.op("dve", lambda e: e.tensor_copy(base[:], incl[:, NT - 1, :]), reads=[incl], writes=[base])
            S.op("dve", lambda e: e.tensor_tensor(out=dst[:], in0=incl[:], in1=pcs[:], op=ALU.subtract), reads=[incl, pcs, dst], writes=[dst])
            for hh in range(2):
                S.op("dve", lambda e, hh=hh, dst=dst: e.tensor_tensor(out=rank[:, hh * 16:(hh + 1) * 16, :].rearrange("p t e -> p (t e)"),
                                                                      in0=ppr[hh][:], in1=dst[:, hh * 16:(hh + 1) * 16, :].rearrange("p t e -> p (t e)"),
                                                                      op=ALU.add), reads=[ppr[hh], dst, rank], writes=[rank])
            nbk = sb(ph, [128, 32], F32, "nbk")
            psb = sb(ph, [128, 33], F32, "psb")
            pstart = sb(ph, [128, 32], F32, "pstart")
            S.op("dve", lambda e: e.tensor_scalar(out=nbk[:], in0=base[:], scalar1=1.0 / RB,
                                                  scalar2=((RB - 1.0) / RB - 0.5 + 0.5 / RB), op0=ALU.mult, op1=ALU.add),
                 reads=[base], writes=[nbk])
            S.op("dve", lambda e: e.tensor_scalar(out=nbk[:], in0=nbk[:], scalar1=MAGIC, scalar2=None, op0=ALU.add),
                 reads=[nbk], writes=[nbk])
            S.op("dve", lambda e: e.tensor_scalar(out=nbk[:], in0=nbk[:], scalar1=-MAGIC, scalar2=None, op0=ALU.add),
                 reads=[nbk], writes=[nbk])
            S.op("dve", lambda e: e.memset(psb[:, 0:1], 0.0), writes=[psb])
            for ex in range(32):
                S.op("dve", lambda e, ex=ex: e.tensor_tensor(out=psb[:, ex + 1:ex + 2], in0=psb[:, ex:ex + 1], in1=nbk[:, ex:ex + 1], op=ALU.add),
                     reads=[psb, nbk], writes=[psb])
            S.op("dve", lambda e: e.tensor_scalar(out=pstart[:], in0=psb[:, 0:32], scalar1=float(RB), scalar2=None, op0=ALU.mult),
                 reads=[psb], writes=[pstart])
            blk_e = sb(ph, [128, NBIG], F32, "blk_e")
            cmp3 = sb(ph, [128, NBIG, 32], F32, "cmp3")
            S.op("dve", lambda e: e.tensor_tensor(out=cmp3[:], in0=psb[:, 1:33].unsqueeze(1).to_broadcast([128, NBIG, 32]),
                                                  in1=iota_b[:, 0:NBIG].unsqueeze(2).to_broadcast([128, NBIG, 32]), op=ALU.is_le),
                 reads=[psb, iota_b], writes=[cmp3])
            S.op("dve", lambda e: e.reduce_sum(out=blk_e[:], in_=cmp3[:], axis=AX.X), reads=[cmp3], writes=[blk_e])
            S.op("dve", lambda e: e.tensor_scalar(out=blk_e[:], in0=blk_e[:], scalar1=31.0, scalar2=None, op0=ALU.min),
                 reads=[blk_e], writes=[blk_e])
            rr = sb(ph, [128, NT, 32], F32, "rr")
            prod = sb(ph, [128, NT, 2, 32], F32, "prod")
            destf = sb(ph, [128, NT, 2], F32, "destf")
            S.op("dve", lambda e: e.tensor_tensor(out=rr[:], in0=rank[:], in1=pstart[:].unsqueeze(1).to_broadcast([128, NT, 32]), op=ALU.add),
                 reads=[rank, pstart], writes=[rr])
            for k in range(2):
                S.op("dve", lambda e, k=k: e.tensor_tensor(out=prod[:, :, k, :], in0=rr[:], in1=MM[:, :, k, :], op=ALU.mult),
                     reads=[rr, MM, prod], writes=[prod])
            S.op("dve", lambda e: e.reduce_sum(out=destf[:], in_=prod[:], axis=AX.X), reads=[prod], writes=[destf])
            S.op("dve", lambda e: e.tensor_copy(desti[:], destf[:]), reads=[destf], writes=[desti])
            wf1 = sb(ph, [128, NBIG], F32, "wf1")
            S.op("dve", lambda e: e.tensor_scalar(out=wf1[:], in0=blk_e[:], scalar1=128.0, scalar2=vec[:, 2:3], op0=ALU.mult, op1=ALU.add),
                 reads=[blk_e, vec], writes=[wf1])
            S.op("dve", lambda e: e.tensor_copy(widx1[:], wf1[:]), reads=[wf1], writes=[widx1])
            for t in range(NT):
                def scat(e, sem, t=t):
                    for k in range(2):
                        e.indirect_dma_start(out=xpad_d[:, :], out_offset=bass.IndirectOffsetOnAxis(ap=desti[:, t, k:k + 1], axis=0),
                                             in_=h2all[:, t, :], in_offset=None).then_inc(sem, 16)
                S.custom("pool", scat, h2all, 2, reads=[h2b[t], desti, B_xz], track=[B_xpad])
            if debug:
                dbg_d = nc.dram_tensor("dbg_d", [128, NBIG + 64 + 64], F32, kind="ExternalOutput").ap()
                dbt = sb(ph, [128, NBIG + 128], F32, "dbt")
                S.op("dve", lambda e: e.tensor_copy(dbt[:, 0:NBIG], blk_e[:]), reads=[blk_e], writes=[dbt])
                S.op("dve", lambda e: e.tensor_copy(dbt[:, NBIG:NBIG + 64], destf[:].rearrange("p t k -> p (t k)")), reads=[destf, dbt], writes=[dbt])
                S.op("dve", lambda e: e.tensor_copy(dbt[:, NBIG + 64:NBIG + 128], wAB[:].rearrange("p t k -> p (t k)")), reads=[wAB, dbt], writes=[dbt])
                S.dma("sp", dbg_d, dbt[:], dbt, reads=[dbt], writes=[Buf("dbg")])
            S.barrier()
            S.emit()
        if stop <= 5:
            S.emit()
            return nc

        with ExitStack() as ph:
            NW = 3 if NSUB == 1 else 2
            W1 = [sb(ph, [128, 8 * D_EXP], BF16, "W1_%d" % i) for i in range(NW)]
            W3 = [sb(ph, [128, 8 * D_EXP], BF16, "W3_%d" % i) for i in range(NW)]
            W2 = [sb(ph, [128, 4 * D], BF16, "W2_%d" % i) for i in range(NW)]
            pXT = [ps(ph, [128, 8, 128], BF16, "pXT%d" % i) for i in range(1)]
            pG = [ps(ph, [128, 512], F32, "pG%d" % i) for i in range(2)]
            pU = [ps(ph, [128, 512], F32, "pU%d" % i) for i in range(2)]
            pAT = [ps(ph, [128, 4, 128], BF16, "pAT%d" % i) for i in range(1)]
            pY = [ps(ph, [128, 512], F32, "pY%d" % i) for i in range(2)]
            sgt = [sb(ph, [128, 512], BF16, "sgt%d" % i) for i in range(2)]
            actt = [sb(ph, [128, 512], BF16, "act%d" % i) for i in range(2)]
            actT = [sb(ph, [128, 4, 128], BF16, "actT%d" % i) for i in range(2)]
            yblk = [sb(ph, [128, D], F32, "yblk%d" % i) for i in range(2)]

            def load_w(bb):
                b = bb
                wi = bb % NW
                for (Wt, rows) in [(W1[wi], wb_eg), (W3[wi], wb_eu), (W2[wi], wb_ed)]:
                    def g1(e, sem, Wt=Wt, rows=rows, b=b):
                        e.indirect_dma_start(out=Wt[:, :], out_offset=None, in_=rows[:, :],
                                             in_offset=bass.IndirectOffsetOnAxis(ap=widx1[:, b:b + 1], axis=0)).then_inc(sem, 16)
                    S.custom("pool", g1, Wt, 1, reads=[widx1, B_wb], writes=[Wt])

            xb4 = [sb(ph, [128, D], BF16, "xb4_%d" % i) for i in range(4)]
            xT3 = [sb(ph, [128, 8, 128], BF16, "xT3_%d" % i) for i in range(3)]

            def stage_x(b):
                x_ = xb4[b % 4]
                S.dma("sp", x_[:], xpad_d[b * 128:(b + 1) * 128, :], x_, reads=[B_xpad], writes=[x_])

            def stage_a(b):
                if b % NSUB == 0:
                    load_w(b // NSUB)
                x_ = xb4[b % 4]
                xT = xT3[b % 3]
                pxt = pXT[0]

                def trx(e):
                    for c in range(8):
                        ins = e.transpose(pxt[:, c, :], x_[:, c:D:8], ident[:])
                    return ins
                S.op("pe", trx, reads=[x_, ident], writes=[pxt])
                S.op("dve", lambda e: e.tensor_copy(xT[:], pxt[:]), reads=[pxt], writes=[xT])

            def stage_b(b):
                wi = (b // NSUB) % NW
                xT = xT3[b % 3]
                pg, pu = pG[b % 2], pU[b % 2]
                for (p, Wt) in [(pg, W1[wi]), (pu, W3[wi])]:
                    def mm(e, p=p, Wt=Wt):
                        for k in range(8):
                            ins = e.matmul(p[:], lhsT=xT[:, k, :], rhs=Wt[:, k * D_EXP:(k + 1) * D_EXP], start=(k == 0), stop=(k == 7))
                        return ins
                    S.op("pe", mm, reads=[xT, Wt], writes=[p])
                s_, a_ = sgt[b % 2], actt[b % 2]
                S.op("act", lambda e: e.activation(out=s_[:], in_=pg[:], func=AF.Silu), reads=[pg], writes=[s_])
                S.op("dve", lambda e: e.tensor_tensor(out=a_[:], in0=pu[:], in1=s_[:], op=ALU.mult), reads=[pu, s_], writes=[a_])

            def stage_c(b):
                wi = (b // NSUB) % NW
                a_ = actt[b % 2]
                pat = pAT[0]
                aT = actT[b % 2]
                yb2 = yblk[b % 2]

                def tr(e):
                    for c in range(4):
                        ins = e.transpose(pat[:, c, :], a_[:, c:D_EXP:4], ident[:])
                    return ins
                S.op("pe", tr, reads=[a_, ident], writes=[pat])
                S.op("act", lambda e: e.activation(out=aT[:], in_=pat[:], func=AF.Copy), reads=[pat], writes=[aT])
                for half in range(2):
                    py = pY[half]
                    hs = slice(half * 512, (half + 1) * 512)

                    def mm(e, py=py, hs=hs):
                        for k in range(4):
                            ins = e.matmul(py[:], lhsT=aT[:, k, :], rhs=W2[wi][:, k * D + hs.start:k * D + hs.stop], start=(k == 0), stop=(k == 3))
                        return ins
                    S.op("pe", mm, reads=[aT, W2[wi]], writes=[py])
                    if half == 0:
                        S.op("dve", lambda e, py=py, hs=hs: e.tensor_copy(yb2[:, hs], py[:]), reads=[py], writes=[yb2])
                    else:
                        S.op("act", lambda e, py=py, hs=hs: e.activation(out=yb2[:, hs], in_=py[:], func=AF.Copy), reads=[py, yb2], writes=[yb2])
                S.dma("act", ypad_d[b * 128:(b + 1) * 128, :], yb2[:], yb2, reads=[yb2], writes=[B_ypad], acc=True)

            for b in range(3):
                stage_x(b)
            stage_a(0)
            stage_a(1)
            stage_b(0)
            for b in range(NBLK):
                if b + 3 < NBLK:
                    stage_x(b + 3)
                if b + 2 < NBLK:
                    stage_a(b + 2)
                if b + 1 < NBLK:
                    stage_b(b + 1)
                stage_c(b)
            S.barrier()
            S.emit()

        with ExitStack() as ph:
            NB_F = 4
            y0t = [sb(ph, [128, D], F32, "y0t%d" % i) for i in range(NB_F)]
            y1t = [sb(ph, [128, D], F32, "y1t%d" % i) for i in range(NB_F)]
            x1t = [sb(ph, [128, D], F32, "x1t%d" % i) for i in range(NB_F)]
            ot = [sb(ph, [128, D], F32, "ot%d" % i) for i in range(NB_F)]
            junk = sb(ph, [128, D], BF16, "junk")
            ssq = [sb(ph, [128, 1], F32, "ss%d" % i) for i in range(NB_F)]

            def fin_load(t):
                x1_ = x1t[t % NB_F]
                ya, yb3 = y0t[t % NB_F], y1t[t % NB_F]
                for k, yt in enumerate([ya, yb3]):
                    def gath(e, sem, yt=yt, t=t, k=k):
                        e.indirect_dma_start(out=yt[:, :], out_offset=None, in_=ypad_d[:, :],
                                             in_offset=bass.IndirectOffsetOnAxis(ap=desti[:, t, k:k + 1], axis=0)).then_inc(sem, 16)
                    S.custom("pool", gath, yt, 1, reads=[desti, B_ypad], writes=[yt])
                S.dma("sp", x1_[:], x1_d[t * 128:(t + 1) * 128, :], x1_, reads=[B_x1], writes=[x1_])

            def fin_a(t):
                o_ = ot[t % NB_F]
                ya, yb3 = y0t[t % NB_F], y1t[t % NB_F]
                S.op("dve", lambda e: e.tensor_scalar(out=o_[:], in0=ya[:], scalar1=wAB[:, t, 0:1], scalar2=None, op0=ALU.mult),
                     reads=[ya, wAB], writes=[o_])
                S.op("dve", lambda e: e.scalar_tensor_tensor(out=o_[:], in0=yb3[:], scalar=wAB[:, t, 1:2], in1=o_[:],
                                                             op0=ALU.mult, op1=ALU.add), reads=[yb3, wAB, o_], writes=[o_])
                S.op("pool", lambda e: e.tensor_tensor(out=o_[:], in0=o_[:], in1=g2b[:], op=ALU.mult), reads=[o_], writes=[o_])

            def fin_b(t):
                x1_ = x1t[t % NB_F]
                o_ = ot[t % NB_F]
                ss = ssq[t % NB_F]
                S.op("dve", lambda e: e.tensor_tensor(out=o_[:], in0=o_[:], in1=x1_[:], op=ALU.add), reads=[o_, x1_], writes=[o_])
                S.op("act", lambda e: e.activation(out=junk[:], in_=o_[:], func=AF.Square, accum_out=ss[:]),
                     reads=[o_], writes=[junk, ss])
                rstd_ops(ss, D)
                S.op("dve", lambda e: e.scalar_tensor_tensor(out=o_[:], in0=o_[:], scalar=ss[:, 0:1], in1=nfb[:],
                                                             op0=ALU.mult, op1=ALU.mult), reads=[o_, ss], writes=[o_])
                S.dma("act", out_d[t * 128:(t + 1) * 128, :], o_[:], o_, reads=[o_], writes=[B_out], acc=True)

            for t in range(min(NB_F - 1, NT)):
                fin_load(t)
            fin_a(0)
            for t in range(NT):
                if t + NB_F - 1 < NT:
                    fin_load(t + NB_F - 1)
                if t + 1 < NT:
                    fin_a(t + 1)
                fin_b(t)
            S.barrier()
        S.emit()
    return nc


def make_in_maps(inputs):
    f = lambda k: np.ascontiguousarray(np.asarray(inputs[k]))
    x = f("x")
    c = f("c")
    pos = f("positions").astype(np.int32)
    aux = np.concatenate([f("b_ada")[0], f("norm_mix")[0], f("norm_ffn")[0], f("norm_final"), f("diff_norm")[0],
                          f("b_router_group")[0], f("b_router_expert")[0]]).astype(np.float32)[None, :]
    lam = np.stack([f("lam_q1")[0], f("lam_k1")[0], f("lam_q2")[0], f("lam_k2")[0]]).astype(np.float32)
    w_rt = np.ascontiguousarray(np.concatenate([f("w_router_group")[0], f("w_router_expert")[0]], axis=1))
    shared = {
        "w_ada": f("w_ada")[0], "aux": aux, "w_in": f("w_in")[0], "w_ret_o": f("w_ret_o")[0], "w_diff_o": f("w_diff_o")[0],
        "w_out": f("w_out")[0], "lam": lam, "w_rt": w_rt, "w_exp_gate": f("w_exp_gate")[0].reshape(N_EXP * 128, 8 * D_EXP), "w_exp_up": f("w_exp_up")[0].reshape(N_EXP * 128, 8 * D_EXP),
        "w_exp_down": f("w_exp_down")[0].reshape(N_EXP * 128, 4 * D),
    }
    for k, v in CONSTS.items():
        if not k.startswith("_"):
            shared[k] = v
    maps = []
    for b in range(x.shape[0]):
        m = dict(shared)
        m["x"] = np.ascontiguousarray(x[b])
        m["c"] = np.ascontiguousarray(c[b].reshape(8, 128).T)
        m["positions"] = np.ascontiguousarray(pos[b][None, :])
        maps.append(m)
    return maps


_NC_CACHE = {}


def kernel(**inputs):
    maps = make_in_maps(inputs)
    if "nc" not in _NC_CACHE:
        _NC_CACHE["nc"] = build()
    nc = _NC_CACHE["nc"]
    res = run_bass_kernel_spmd(nc, maps, core_ids=list(range(8)))
    out = np.stack([np.asarray(r["out"]) for r in res.results], axis=0)
    return out.astype(np.float32)
```
